# Optimizing a Trainium2 kernel written in Bass

```python
import math
import jax
import jax.numpy as jnp
from jax import lax
import numpy as np

D_MODEL = 2048
BATCH = 4
SEQ = 2048
DEPTH = 4

GRID_W = 64
CTX_LEN = 256
EPS = 1e-6
N_EVEN = (DEPTH + 1) // 2
N_ODD = DEPTH // 2
FNET_HEADS = 4
FNET_DIM = D_MODEL // 16
FNET_WIDTH = FNET_HEADS * FNET_DIM
S5_WIDTH = D_MODEL - FNET_WIDTH
S5_GROUP = 16
S5_GROUPS = S5_WIDTH // S5_GROUP
S5_STATE = 64
S5_DT_MIN = 0.001
S5_DT_MAX = 0.1
POOL_WINDOWS = (2, 4, 8, 16)
POOL_GROUPS = len(POOL_WINDOWS)
POOL_DIM = D_MODEL // 16
POOL_WIDTH = POOL_GROUPS * POOL_DIM
HEAD_DIM = 128
ATT_WIDTH = D_MODEL - POOL_WIDTH
ATT_HEADS = ATT_WIDTH // HEAD_DIM
KV_HEADS = 4
Q_PER_KV = ATT_HEADS // KV_HEADS
KV_WIDTH = KV_HEADS * HEAD_DIM
KV_OFFSET = POOL_WIDTH + ATT_WIDTH
ODD_IN_WIDTH = KV_OFFSET + 2 * KV_WIDTH
Q_BLOCK = 128
ROPE_THETA = 10000.0
ROPE_FREQS = HEAD_DIM // 4
ATT_SCALE = HEAD_DIM ** -0.5
N_EXPERTS = 32
TOP_K = 4
EXPERT_FF = 512
SWIGLU_LIMIT = 7.0
SWIGLU_ALPHA = 1.702
MOE_BLOCK = 128

kernel_name = 'hybrid_s5_fnet_pool_gqa_moe_dit'

F32 = jnp.float32
C64 = jnp.complex64


def rmsnorm(x, g):
    xf = x.astype(F32)
    y = xf * lax.rsqrt(jnp.mean(xf * xf, axis=-1, keepdims=True) + EPS)
    return (y * g.astype(F32)).astype(x.dtype)


def adaln(cond, w, b):
    m = jnp.dot(jax.nn.silu(cond), w) + b
    return [t[:, None, :] for t in jnp.split(m, 6, axis=-1)]


def modulate(h, shift, scale):
    return h * (1 + scale) + shift


def s5_discretize(lam_re, lam_im, log_dt, b_re, b_im, c_re, c_im):
    lam = lax.complex(lam_re.astype(F32), lam_im.astype(F32))
    dt = jnp.exp(log_dt.astype(F32))[..., None]
    lam_bar = jnp.exp(lam * dt)
    b = lax.complex(b_re.astype(F32), b_im.astype(F32))
    b_bar = ((lam_bar - 1.0) / lam)[..., None] * b
    c_mat = lax.complex(c_re.astype(F32), c_im.astype(F32))
    return lam_bar, b_bar, c_mat


def scan_combine(left, right):
    a_l, b_l = left
    a_r, b_r = right
    return a_l * a_r, a_r * b_l + b_r


def diag_scan(lam_bar, bu, h0):
    if h0 is not None:
        bu = bu.at[:, 0].add(lam_bar * h0)
    a = jnp.broadcast_to(lam_bar, bu.shape)
    return lax.associative_scan(scan_combine, (a, bu), axis=1)[1]


def s5_direction(u_lat, u_ctx, lam_bar, b_bar, c_mat, reverse, need_ctx_out):
    flip = (lambda z: jnp.flip(z, axis=1)) if reverse else (lambda z: z)
    bu_ctx = jnp.einsum('btgc,gpc->btgp', flip(u_ctx).astype(C64), b_bar)
    h_ctx = diag_scan(lam_bar, bu_ctx, None)
    bu_lat = jnp.einsum('btgc,gpc->btgp', flip(u_lat).astype(C64), b_bar)
    h_lat = diag_scan(lam_bar, bu_lat, h_ctx[:, -1])
    y_lat = flip(jnp.einsum('btgp,gcp->btgc', h_lat, c_mat).real)
    y_ctx = flip(jnp.einsum('btgp,gcp->btgc', h_ctx, c_mat).real) if need_ctx_out else None
    return y_lat, y_ctx


def s5_output(y_ssm, u, d_skip, glu_w, glu_b):
    y = y_ssm + d_skip.astype(F32) * u.astype(F32)
    g = jax.nn.gelu(y).astype(u.dtype)
    return g * jax.nn.sigmoid(g @ glu_w + glu_b)


def fourier_mix(f, fnet_w):
    spec = jnp.fft.fft2(f.astype(F32), axes=(1, 3), norm='ortho').real.astype(f.dtype)
    return jnp.einsum('bthd,hde->bthe', spec, fnet_w)


def s5_fourier_mixer(h_lat, h_ctx, w_in, w_out, lam_re, lam_im, log_dt, b_re, b_im, c_re, c_im,
                     d_skip, glu_w, glu_b, fnet_w, need_ctx_out):
    B, S, _ = h_lat.shape
    L = h_ctx.shape[1]
    lam_bar, b_bar, c_mat = s5_discretize(lam_re, lam_im, log_dt, b_re, b_im, c_re, c_im)
    p_lat = h_lat @ w_in
    u_lat, f_lat = p_lat[..., :S5_WIDTH], p_lat[..., S5_WIDTH:]
    if need_ctx_out:
        p_ctx = h_ctx @ w_in
        u_ctx, f_ctx = p_ctx[..., :S5_WIDTH], p_ctx[..., S5_WIDTH:]
    else:
        u_ctx = h_ctx @ w_in[:, :S5_WIDTH]
    ul = u_lat.astype(F32).reshape(B, S, S5_GROUPS, S5_GROUP)
    uc = u_ctx.astype(F32).reshape(B, L, S5_GROUPS, S5_GROUP)
    yf_lat, yf_ctx = s5_direction(ul, uc, lam_bar[0], b_bar[0], c_mat[0], False, need_ctx_out)
    yb_lat, yb_ctx = s5_direction(ul, uc, lam_bar[1], b_bar[1], c_mat[1], True, need_ctx_out)

    def merge(y_ssm, u, f, n):
        a = s5_output(y_ssm.reshape(B, n, S5_WIDTH), u, d_skip, glu_w, glu_b)
        b = fourier_mix(f.reshape(B, n, FNET_HEADS, FNET_DIM), fnet_w).reshape(B, n, FNET_WIDTH)
        return jnp.concatenate([a, b], axis=-1) @ w_out

    out_lat = merge(yf_lat + yb_lat, u_lat, f_lat, S)
    out_ctx = merge(yf_ctx + yb_ctx, u_ctx, f_ctx, L) if need_ctx_out else None
    return out_lat, out_ctx


def centred_pool_minus_self(v, window):
    n = v.shape[1]
    vf = v.astype(F32)
    cs = jnp.concatenate([jnp.zeros_like(vf[:, :1]), lax.cumsum(vf, axis=1)], axis=1)
    t = jnp.arange(n)
    lo = jnp.clip(t - window // 2, 0, n)
    hi = jnp.clip(t + window - window // 2, 0, n)
    mean = (jnp.take(cs, hi, axis=1) - jnp.take(cs, lo, axis=1)) / (hi - lo).astype(F32)[None, :, None]
    return mean - vf


def pool_mix(v, pool_w, pool_scale):
    B, T, _ = v.shape
    groups = jnp.split(v, POOL_GROUPS, axis=-1)
    pooled = jnp.stack([centred_pool_minus_self(gv, w) for gv, w in zip(groups, POOL_WINDOWS)], axis=2)
    y = jnp.einsum('btgd,gde->btge', pooled.astype(v.dtype), pool_w).reshape(B, T, POOL_WIDTH)
    return y * pool_scale


def axial_rope_tables(n_tok):
    rows = n_tok // GRID_W
    row = jnp.repeat(jnp.arange(rows), GRID_W)
    col = jnp.tile(jnp.arange(GRID_W), rows)
    pos = jnp.stack([row, col], axis=-1).astype(F32)
    inv = jnp.power(ROPE_THETA, -jnp.arange(ROPE_FREQS, dtype=F32) / ROPE_FREQS)
    ang = pos[:, :, None] * inv
    return jnp.cos(ang), jnp.sin(ang)


def apply_axial_rope(x, cos, sin):
    B, T, H, Dh = x.shape
    xr = x.astype(F32).reshape(B, T, H, 2, 2, ROPE_FREQS)
    x1, x2 = xr[..., 0, :], xr[..., 1, :]
    c = cos[None, :, None]
    s = sin[None, :, None]
    out = jnp.stack([x1 * c - x2 * s, x2 * c + x1 * s], axis=-2)
    return out.reshape(B, T, H, Dh).astype(x.dtype)


def latent_attention(q, k, v, k_ctx, v_ctx):
    B, S, _, _ = q.shape
    k_all = jnp.concatenate([k_ctx, k], axis=1)
    v_all = jnp.concatenate([v_ctx, v], axis=1)
    n_blk = S // Q_BLOCK
    qb = q.reshape(B, n_blk, Q_BLOCK, KV_HEADS, Q_PER_KV, HEAD_DIM).transpose(1, 0, 2, 3, 4, 5)

    def one_block(q_blk):
        s = jnp.einsum('bqhgd,bkhd->bhgqk', q_blk, k_all, preferred_element_type=F32) * ATT_SCALE
        p = jax.nn.softmax(s, axis=-1)
        return jnp.einsum('bhgqk,bkhd->bqhgd', p.astype(v_all.dtype), v_all, preferred_element_type=F32)

    o = lax.map(one_block, qb)
    return o.transpose(1, 0, 2, 3, 4, 5).reshape(B, S, ATT_WIDTH).astype(q.dtype)


def context_attention(q, k, v):
    B, L, _, _ = q.shape
    qg = q.reshape(B, L, KV_HEADS, Q_PER_KV, HEAD_DIM)
    s = jnp.einsum('bqhgd,bkhd->bhgqk', qg, k, preferred_element_type=F32) * ATT_SCALE
    p = jax.nn.softmax(s, axis=-1)
    o = jnp.einsum('bhgqk,bkhd->bqhgd', p.astype(v.dtype), v, preferred_element_type=F32)
    return o.reshape(B, L, ATT_WIDTH).astype(q.dtype)


def pool_attention_mixer(h_lat, h_ctx, w_in, w_out, pool_w, pool_scale, q_gain, k_gain, cos, sin, need_ctx_out):
    B, S, _ = h_lat.shape
    L = h_ctx.shape[1]
    p_lat = h_lat @ w_in
    pool_lat = p_lat[..., :POOL_WIDTH]
    q_lat = p_lat[..., POOL_WIDTH:KV_OFFSET].reshape(B, S, ATT_HEADS, HEAD_DIM)
    k_lat = p_lat[..., KV_OFFSET:KV_OFFSET + KV_WIDTH].reshape(B, S, KV_HEADS, HEAD_DIM)
    v_lat = p_lat[..., KV_OFFSET + KV_WIDTH:].reshape(B, S, KV_HEADS, HEAD_DIM)
    q_lat = apply_axial_rope(rmsnorm(q_lat, q_gain), cos, sin)
    k_lat = apply_axial_rope(rmsnorm(k_lat, k_gain), cos, sin)
    kv_ctx = h_ctx @ w_in[:, KV_OFFSET:]
    k_ctx = rmsnorm(kv_ctx[..., :KV_WIDTH].reshape(B, L, KV_HEADS, HEAD_DIM), k_gain)
    v_ctx = kv_ctx[..., KV_WIDTH:].reshape(B, L, KV_HEADS, HEAD_DIM)
    att_lat = latent_attention(q_lat, k_lat, v_lat, k_ctx, v_ctx)
    out_lat = jnp.concatenate([pool_mix(pool_lat, pool_w, pool_scale), att_lat], axis=-1) @ w_out
    if not need_ctx_out:
        return out_lat, None
    pq_ctx = h_ctx @ w_in[:, :KV_OFFSET]
    q_ctx = rmsnorm(pq_ctx[..., POOL_WIDTH:].reshape(B, L, ATT_HEADS, HEAD_DIM), q_gain)
    att_ctx = context_attention(q_ctx, k_ctx, v_ctx)
    out_ctx = jnp.concatenate([pool_mix(pq_ctx[..., :POOL_WIDTH], pool_w, pool_scale), att_ctx], axis=-1) @ w_out
    return out_lat, out_ctx


def moe_ffn(h, w_router, b_router, w_gate, b_gate, w_up, b_up, w_down, b_down):
    n_tok, d = h.shape
    logits = jnp.dot(h, w_router, preferred_element_type=F32) + b_router.astype(F32)
    top_val, top_idx = lax.top_k(logits, TOP_K)
    top_w = jax.nn.softmax(top_val, axis=-1)
    n_assign = n_tok * TOP_K
    cap = -(-(n_assign + N_EXPERTS * (MOE_BLOCK - 1)) // MOE_BLOCK) * MOE_BLOCK
    n_blocks = cap // MOE_BLOCK
    flat_e = top_idx.reshape(-1)
    flat_tok = jnp.repeat(jnp.arange(n_tok, dtype=jnp.int32), TOP_K)
    flat_w = top_w.reshape(-1)
    order = jnp.argsort(flat_e)
    sorted_e = flat_e[order]
    counts = jnp.bincount(flat_e, length=N_EXPERTS)
    start = jnp.cumsum(counts) - counts
    padded = (counts + MOE_BLOCK - 1) // MOE_BLOCK * MOE_BLOCK
    pend = jnp.cumsum(padded)
    pstart = pend - padded
    dest = pstart[sorted_e] + (jnp.arange(n_assign) - start[sorted_e])
    tok_buf = jnp.full((cap,), n_tok, jnp.int32).at[dest].set(flat_tok[order])
    w_buf = jnp.zeros((cap,), F32).at[dest].set(flat_w[order])
    blk_e = jnp.clip(jnp.searchsorted(pend, jnp.arange(n_blocks) * MOE_BLOCK, side='right'), 0, N_EXPERTS - 1)
    h_pad = jnp.concatenate([h, jnp.zeros((1, d), h.dtype)], axis=0)

    def expert_block(args):
        tok, wgt, e = args
        xb = h_pad[tok]
        g = jnp.minimum(xb @ w_gate[e] + b_gate[e], SWIGLU_LIMIT)
        u = jnp.clip(xb @ w_up[e] + b_up[e], -SWIGLU_LIMIT, SWIGLU_LIMIT)
        a = g * jax.nn.sigmoid(SWIGLU_ALPHA * g) * (u + 1)
        return ((a @ w_down[e] + b_down[e]) * wgt[:, None]).astype(h.dtype)

    rows = lax.map(expert_block, (tok_buf.reshape(n_blocks, MOE_BLOCK), w_buf.reshape(n_blocks, MOE_BLOCK), blk_e))
    out = jnp.zeros_like(h_pad).at[tok_buf].add(rows.reshape(cap, d))
    return out[:n_tok]


def setup_inputs(seed: int = 0) -> dict:
    key = jax.random.key(seed)
    ks = iter(jax.random.split(key, 64))
    D, G, P, C, E, F = D_MODEL, S5_GROUPS, S5_STATE, S5_GROUP, N_EXPERTS, EXPERT_FF

    def nrm(shape, scale):
        return jax.random.normal(next(ks), shape, F32) * scale

    return {
        'x': nrm((BATCH, SEQ, D), 1.0),
        'c': nrm((BATCH, D), 1.0),
        'ctx': nrm((BATCH, CTX_LEN, D), 1.0),
        'c_ctx': nrm((D,), 1.0),
        'ada_w': nrm((DEPTH, D, 6 * D), 0.5 * D ** -0.5),
        'ada_b': nrm((DEPTH, 6 * D), 0.02),
        'norm_mix_g': 1.0 + nrm((DEPTH, D), 0.02),
        'norm_ffn_g': 1.0 + nrm((DEPTH, D), 0.02),
        'ev_w_in': nrm((N_EVEN, D, D), D ** -0.5),
        'ev_w_out': nrm((N_EVEN, D, D), D ** -0.5),
        's5_lam_re': -0.5 + nrm((N_EVEN, 2, G, P), 0.01),
        's5_lam_im': math.pi * jnp.arange(P, dtype=F32) + nrm((N_EVEN, 2, G, P), 0.01),
        's5_log_dt': jax.random.uniform(next(ks), (N_EVEN, 2, G), F32, math.log(S5_DT_MIN), math.log(S5_DT_MAX)),
        's5_b_re': nrm((N_EVEN, 2, G, P, C), (2 * C) ** -0.5),
        's5_b_im': nrm((N_EVEN, 2, G, P, C), (2 * C) ** -0.5),
        's5_c_re': nrm((N_EVEN, 2, G, C, P), P ** -0.5),
        's5_c_im': nrm((N_EVEN, 2, G, C, P), P ** -0.5),
        's5_d': nrm((N_EVEN, S5_WIDTH), 1.0),
        's5_glu_w': nrm((N_EVEN, S5_WIDTH, S5_WIDTH), S5_WIDTH ** -0.5),
        's5_glu_b': nrm((N_EVEN, S5_WIDTH), 0.02),
        'fnet_w': nrm((N_EVEN, FNET_HEADS, FNET_DIM, FNET_DIM), FNET_DIM ** -0.5),
        'od_w_in': nrm((N_ODD, D, ODD_IN_WIDTH), D ** -0.5),
        'od_w_out': nrm((N_ODD, D, D), D ** -0.5),
        'pool_w': nrm((N_ODD, POOL_GROUPS, POOL_DIM, POOL_DIM), POOL_DIM ** -0.5),
        'pool_scale': 1.0 + nrm((N_ODD, POOL_WIDTH), 0.1),
        'q_gain': 1.0 + nrm((N_ODD, HEAD_DIM), 0.02),
        'k_gain': 1.0 + nrm((N_ODD, HEAD_DIM), 0.02),
        'router_w': nrm((DEPTH, D, E), D ** -0.5),
        'router_b': nrm((DEPTH, E), 0.01),
        'exp_w_gate': nrm((DEPTH, E, D, F), D ** -0.5),
        'exp_b_gate': nrm((DEPTH, E, F), 0.02),
        'exp_w_up': nrm((DEPTH, E, D, F), D ** -0.5),
        'exp_b_up': nrm((DEPTH, E, F), 0.02),
        'exp_w_down': nrm((DEPTH, E, F, D), F ** -0.5),
        'exp_b_down': nrm((DEPTH, E, D), 0.02),
        'final_g': 1.0 + nrm((D,), 0.02),
    }


def reference(x, c, ctx, c_ctx, ada_w, ada_b, norm_mix_g, norm_ffn_g, ev_w_in, ev_w_out,
              s5_lam_re, s5_lam_im, s5_log_dt, s5_b_re, s5_b_im, s5_c_re, s5_c_im, s5_d, s5_glu_w, s5_glu_b,
              fnet_w, od_w_in, od_w_out, pool_w, pool_scale, q_gain, k_gain, router_w, router_b,
              exp_w_gate, exp_b_gate, exp_w_up, exp_b_up, exp_w_down, exp_b_down, final_g):
    B, S, D = x.shape
    cos, sin = axial_rope_tables(S)
    for layer in range(DEPTH):
        need_ctx_out = layer < DEPTH - 1
        i = layer // 2
        sh1, sc1, g1, sh2, sc2, g2 = adaln(c, ada_w[layer], ada_b[layer])
        csh1, csc1, cg1, csh2, csc2, cg2 = adaln(c_ctx[None], ada_w[layer], ada_b[layer])
        h_lat = modulate(rmsnorm(x, norm_mix_g[layer]), sh1, sc1)
        h_ctx = modulate(rmsnorm(ctx, norm_mix_g[layer]), csh1, csc1)
        if layer % 2 == 0:
            y_lat, y_ctx = s5_fourier_mixer(h_lat, h_ctx, ev_w_in[i], ev_w_out[i], s5_lam_re[i], s5_lam_im[i],
                                            s5_log_dt[i], s5_b_re[i], s5_b_im[i], s5_c_re[i], s5_c_im[i],
                                            s5_d[i], s5_glu_w[i], s5_glu_b[i], fnet_w[i], need_ctx_out)
        else:
            y_lat, y_ctx = pool_attention_mixer(h_lat, h_ctx, od_w_in[i], od_w_out[i], pool_w[i], pool_scale[i],
                                                q_gain[i], k_gain[i], cos, sin, need_ctx_out)
        x = x + g1 * y_lat
        h_lat = modulate(rmsnorm(x, norm_ffn_g[layer]), sh2, sc2)
        moe_args = (router_w[layer], router_b[layer], exp_w_gate[layer], exp_b_gate[layer],
                    exp_w_up[layer], exp_b_up[layer], exp_w_down[layer], exp_b_down[layer])
        if need_ctx_out:
            ctx = ctx + cg1 * y_ctx
            h_ctx = modulate(rmsnorm(ctx, norm_ffn_g[layer]), csh2, csc2)
            n_lat = B * S
            out = moe_ffn(jnp.concatenate([h_lat.reshape(-1, D), h_ctx.reshape(-1, D)], axis=0), *moe_args)
            x = x + g2 * out[:n_lat].reshape(x.shape)
            ctx = ctx + cg2 * out[n_lat:].reshape(ctx.shape)
        else:
            x = x + g2 * moe_ffn(h_lat.reshape(-1, D), *moe_args).reshape(x.shape)
    return rmsnorm(x, final_g)
```

```python
import contextlib
import math
import numpy as np
import concourse.bass as bass
import concourse.mybir as mybir
from concourse.bass_utils import run_bass_kernel_spmd

F32 = mybir.dt.float32
BF16 = mybir.dt.bfloat16
AF = mybir.ActivationFunctionType
ALU = mybir.AluOpType
AX = mybir.AxisListType

ENG = ("sync", "act", "dve", "pool", "pe")
NDMA = {"sync": 12, "pool": 8, "act": 6}


class Cfg:
    D = 2048
    B = 4
    S = 2048
    L = 256
    GRID_W = 64
    DEPTH = 4
    E = 32
    TOPK = 4
    FF = 512
    EPS = 1e-6
    S5W = 1536
    NG = 96
    NP = 64
    POOL_WINDOWS = (2, 4, 8, 16)
    HD = 128
    NQH = 12
    NKV = 4
    ODD_IN = 3072
    ROPE_THETA = 10000.0
    LIMIT = 7.0
    ALPHA = 1.702

    @property
    def T(self):
        return self.L + self.S

    @property
    def KC(self):
        return self.D // 128

    @property
    def NE(self):
        return (self.DEPTH + 1) // 2

    @property
    def NO(self):
        return self.DEPTH // 2


class StopBuild(Exception):
    pass


class Prog:
    def __init__(self):
        self.nc = bass.Bass("TRN2", target_bir_lowering=False)
        self.g = contextlib.ExitStack()
        names = ["c_act", "c_dve", "c_pool", "c_pe"]
        for e, n in NDMA.items():
            names += [f"d_{e}_{i}" for i in range(n)]
        self.sems = {n: self.g.enter_context(self.nc.semaphore(n)) for n in names}
        self.bar = self.g.enter_context(self.nc.semaphore("bar"))
        self.bar2 = self.g.enter_context(self.nc.semaphore("bar2"))
        self.rounds = 0
        self.reset_at = 10 ** 12
        self.cnt = {n: 0 for n in names}
        self.lastw = {}
        self.readers = {}
        self.known = {e: {} for e in ENG}
        self.q = {e: [] for e in ENG}
        self.ndma = {e: 0 for e in NDMA}
        self.pst = []
        self.uid = 0
        self.ninst = 0

    def gsb(self, name, shape, dt):
        return self.g.enter_context(self.nc.sbuf_tensor(name, list(shape), dt))

    def sb(self, name, shape, dt):
        self.uid += 1
        return self.pst[-1].enter_context(self.nc.sbuf_tensor(f"{name}_{self.uid}", list(shape), dt))

    def ps(self, name, shape, dt):
        self.uid += 1
        return self.pst[-1].enter_context(self.nc.psum_tensor(f"{name}_{self.uid}", list(shape), dt))

    @contextlib.contextmanager
    def phase(self):
        self.pst.append(contextlib.ExitStack())
        try:
            try:
                yield
            except StopBuild:
                self.flush()
                raise
            self.flush()
        finally:
            self.pst.pop().close()

    def emit(self, eng, fn, reads=(), writes=(), dma=False, noinc=False):
        if dma:
            slot = self.ndma[eng] % NDMA[eng]
            self.ndma[eng] += 1
            sem = f"d_{eng}_{slot}"
            inc = 16
        else:
            sem = "c_" + eng
            inc = 1
        waits = {}

        def need(sv):
            if sv is None:
                return
            s, v = sv
            if v > waits.get(s, 0):
                waits[s] = v
        for b in reads:
            need(self.lastw.get(b))
        for b in writes:
            need(self.lastw.get(b))
            for s, v in self.readers.get(b, {}).items():
                need((s, v))
        if dma and self.cnt[sem] > 0:
            need((sem, self.cnt[sem]))
        kn = self.known[eng]
        wl = []
        own = "c_" + eng
        for s, v in waits.items():
            if s == own and v > self.cnt[own]:
                continue
            if kn.get(s, 0) < v:
                kn[s] = v
                wl.append((s, v))
        if noinc:
            val = self.cnt[sem] + inc
            self.q[eng].append((wl, fn, sem, 0))
        else:
            self.cnt[sem] += inc
            val = self.cnt[sem]
            self.q[eng].append((wl, fn, sem, inc))
        for b in writes:
            self.lastw[b] = (sem, val)
            self.readers[b] = {}
        for b in reads:
            self.readers.setdefault(b, {})[sem] = val
        self.ninst += 1

    def flush(self):
        for e in ENG:
            wl = []
            for s, v in self.cnt.items():
                if v > self.known[e].get(s, 0):
                    self.known[e][s] = v
                    wl.append((s, v))
            self.q[e].append((wl, None, None, 0))
        q = self.q
        sems = self.sems
        with self.nc.Block() as block:
            def replay(name):
                def run(e):
                    for wl, fn, sem, inc in q[name]:
                        for s, v in wl:
                            e.wait_ge(sems[s], v)
                        if fn is not None:
                            ins = fn(e)
                            if inc:
                                ins.then_inc(sems[sem], inc)
                return run
            block.sync(replay("sync"))
            block.scalar(replay("act"))
            block.vector(replay("dve"))
            block.gpsimd(replay("pool"))
            block.tensor(replay("pe"))
        self.q = {e: [] for e in ENG}
        self.lastw.clear()
        self.readers.clear()
        if max(self.cnt.values()) > self.reset_at:
            self.reset_sems()

    def reset_sems(self):
        self.rounds += 1
        r = self.rounds
        sems, bar, bar2 = self.sems, self.bar, self.bar2
        names = list(self.cnt)
        with self.nc.Block() as block:
            def arrive(e):
                e.nop().then_inc(bar, 1)

            def leader(e):
                e.nop().then_inc(bar, 1)
                e.wait_ge(bar, 5 * r)
                for n in names:
                    e.sem_clear(sems[n])
                e.nop().then_inc(bar2, 1)
                e.wait_ge(bar2, r)

            def follower(e):
                arrive(e)
                e.wait_ge(bar2, r)
            block.sync(leader)
            block.scalar(follower)
            block.vector(follower)
            block.gpsimd(follower)
            block.tensor(follower)
        for n in names:
            self.cnt[n] = 0
        self.known = {e: {} for e in ENG}

    def dma(self, eng, out, in_, reads, writes, **kw):
        self.emit(eng, lambda e, o=out, i=in_, kw=kw: e.dma_start(out=o, in_=i, **kw), reads, writes, dma=True)

    def mm(self, out, lhsT, rhs, start, stop, reads, writes, lazy=False):
        self.emit("pe", lambda e, o=out, l=lhsT, r=rhs, a=start, b=stop: e.matmul(o, lhsT=l, rhs=r, start=a, stop=b),
                  reads, writes, noinc=(lazy and not stop))

    def tr(self, out, in_, ident, reads, writes):
        self.emit("pe", lambda e, o=out, i=in_, d=ident: e.transpose(o, i, d), reads, writes)

    def act(self, out, in_, func, reads, writes, bias=None, scale=None, accum_out=None):
        kw = {}
        if bias is not None:
            kw["bias"] = bias
        if scale is not None:
            kw["scale"] = scale
        if accum_out is not None:
            kw["accum_out"] = accum_out
        self.emit("act", lambda e, o=out, i=in_, f=func, kw=kw: e.activation(out=o, in_=i, func=f, **kw), reads, writes)

    def ts(self, eng, out, in0, s1, s2, op0, op1, reads, writes):
        if op1 is None:
            self.emit(eng, lambda e, o=out, i=in0, a=s1, p=op0: e.tensor_scalar(out=o, in0=i, scalar1=a, scalar2=None, op0=p),
                      reads, writes)
        else:
            self.emit(eng, lambda e, o=out, i=in0, a=s1, b=s2, p=op0, q=op1:
                      e.tensor_scalar(out=o, in0=i, scalar1=a, scalar2=b, op0=p, op1=q), reads, writes)

    def tt(self, eng, out, in0, in1, op, reads, writes):
        self.emit(eng, lambda e, o=out, a=in0, b=in1, p=op: e.tensor_tensor(out=o, in0=a, in1=b, op=p), reads, writes)

    def stt(self, out, in0, scalar, in1, op0, op1, reads, writes):
        self.emit("dve", lambda e, o=out, a=in0, s=scalar, b=in1, p=op0, q=op1:
                  e.scalar_tensor_tensor(out=o, in0=a, scalar=s, in1=b, op0=p, op1=q), reads, writes)

    def copy(self, eng, out, in_, reads, writes):
        if eng == "act":
            self.emit("act", lambda e, o=out, i=in_: e.copy(out=o, in_=i), reads, writes)
        else:
            self.emit(eng, lambda e, o=out, i=in_: e.tensor_copy(out=o, in_=i), reads, writes)

    def memset(self, eng, ap, val, writes):
        self.emit(eng, lambda e, a=ap, v=val: e.memset(a, v), (), writes)


def tok_tiles(lo, hi, L, nmax=512):
    out = []
    segs = []
    if lo < L:
        segs.append((lo, min(hi, L)))
    if hi > L:
        segs.append((max(lo, L), hi))
    for a, b in segs:
        t = a
        while t < b:
            n = min(nmax, b - t)
            out.append((t, n))
            t += n
    return out


def build(cfg):
    P = Prog()
    P.reset_at = getattr(cfg, "RESET_AT", 10 ** 12)
    STAGE = getattr(cfg, "STAGE", None)
    nc = P.nc
    D, T, L, S, KC = cfg.D, cfg.T, cfg.L, cfg.S, cfg.KC
    DEPTH, NE, NO, E, FF = cfg.DEPTH, cfg.NE, cfg.NO, cfg.E, cfg.FF
    FC = FF // 128
    NPAIR = cfg.NG // 2
    SC = cfg.S5W // 128
    TCH = T // 128

    def din(name, shape):
        return nc.dram_tensor(name, list(shape), F32, kind="ExternalInput").ap()

    I = {}
    I["xT"] = din("xT", [D, T])
    I["condT"] = din("condT", [128, KC * 2])
    I["ada_w"] = din("ada_w", [DEPTH, D, 6 * D])
    I["ada_bT"] = din("ada_bT", [128, DEPTH * 96])
    I["gmixT"] = din("gmixT", [128, DEPTH * KC])
    I["gffnT"] = din("gffnT", [128, DEPTH * KC])
    I["finalgT"] = din("finalgT", [128, KC])
    if NE:
        I["ev_w_in"] = din("ev_w_in", [NE, D, D])
        I["ev_w_out"] = din("ev_w_out", [NE, D, D])
        I["lamreT"] = din("lamreT", [128, NE * 2 * NPAIR])
        I["lamimT"] = din("lamimT", [128, NE * 2 * NPAIR])
        I["logdtT"] = din("logdtT", [128, NE * 2 * NPAIR])
        I["bblk_re"] = din("bblk_re", [NE * 2 * NPAIR, 128, 128])
        I["bblk_im"] = din("bblk_im", [NE * 2 * NPAIR, 128, 128])
        I["cblk_re"] = din("cblk_re", [NE * 2 * NPAIR, 128, 32])
        I["cblk_im"] = din("cblk_im", [NE * 2 * NPAIR, 128, 32])
        I["s5dT"] = din("s5dT", [128, NE * SC])
        I["glu_w"] = din("glu_w", [NE, cfg.S5W, cfg.S5W])
        I["glu_bT"] = din("glu_bT", [128, NE * SC])
        I["fnet_w"] = din("fnet_w", [NE * 4, 128, 128])
        I["dft_d"] = din("dft_d", [128, 256])
        I["dftc_lat"] = din("dftc_lat", [S, S])
        I["dfts_lat"] = din("dfts_lat", [S, S])
        I["dftc_ctx"] = din("dftc_ctx", [L, L])
        I["dfts_ctx"] = din("dfts_ctx", [L, L])
    if NO:
        I["od_w_in"] = din("od_w_in", [NO, D, cfg.ODD_IN])
        I["od_w_out"] = din("od_w_out", [NO, D, D])
        I["pool_w"] = din("pool_w", [NO * 4, 128, 128])
        I["pool_scaleT"] = din("pool_scaleT", [128, NO * 4])
        I["qgainT"] = din("qgainT", [128, NO])
        I["kgainT"] = din("kgainT", [128, NO])
        I["ropeC"] = din("ropeC", [128, S])
        I["ropeS"] = din("ropeS", [128, S])
        I["ropeR"] = din("ropeR", [128, 128])
        I["pool_rc"] = din("pool_rc", [8, max(S, L)])
    I["router_w"] = din("router_w", [DEPTH, D, E])
    I["router_b"] = din("router_b", [1, DEPTH * E])
    I["w_gate"] = din("w_gate", [DEPTH * E, D, FF])
    I["w_up"] = din("w_up", [DEPTH * E, D, FF])
    I["w_down"] = din("w_down", [DEPTH * E, FF, D])
    I["b_gateT"] = din("b_gateT", [128, DEPTH * E * FC])
    I["b_upT"] = din("b_upT", [128, DEPTH * E * FC])
    I["b_down"] = din("b_down", [DEPTH * E, D])
    outT = nc.dram_tensor("outT", [D, S], F32, kind="ExternalOutput").ap()

    xs = nc.dram_tensor("xs", [D, T], F32).ap()
    uT_d = nc.dram_tensor("uT_d", [D, T], F32).ap()
    yT_d = nc.dram_tensor("yT_d", [cfg.S5W, T], F32).ap()
    gT_d = nc.dram_tensor("gT_d", [E, T], F32).ap()
    pT_d = nc.dram_tensor("pT_d", [2560, T], F32).ap()
    vtok_d = nc.dram_tensor("vtok_d", [T, 512], BF16).ap()

    modT = P.gsb("modT", [128, DEPTH * 96 * 2], F32)
    ident_f = P.gsb("ident_f", [128, 128], F32)
    ident_b = P.gsb("ident_b", [128, 128], BF16)
    ones_f = P.gsb("ones_f", [128, 128], F32)
    gmix = P.gsb("gmix", [128, DEPTH * KC], F32)
    gffn = P.gsb("gffn", [128, DEPTH * KC], F32)
    gfin = P.gsb("gfin", [128, KC], F32)

    def mod(l, v, k, c):
        idx = ((l * 6 + v) * KC + k) * 2 + c
        return modT[:, idx:idx + 1]

    with P.phase():
        iot = P.sb("iot", [128, 128], F32)
        pidx = P.sb("pidx", [128, 1], F32)
        P.emit("pool", lambda e: e.iota(iot[:], pattern=[[1, 128]], base=0, channel_multiplier=0,
                                        allow_small_or_imprecise_dtypes=True), (), ["iot"])
        P.emit("pool", lambda e: e.iota(pidx[:], pattern=[[0, 1]], base=0, channel_multiplier=1,
                                        allow_small_or_imprecise_dtypes=True), (), ["pidx"])
        P.ts("dve", ident_f[:], iot[:], pidx[:, 0:1], None, ALU.is_equal, None, ["iot", "pidx"], ["ident_f"])
        P.copy("dve", ident_b[:], ident_f[:], ["ident_f"], ["ident_b"])
        P.memset("dve", ones_f[:], 1.0, ["ones_f"])
        P.dma("sync", gmix[:], I["gmixT"][:, :], (), ["gmix"])
        P.dma("sync", gffn[:], I["gffnT"][:, :], (), ["gffn"])
        P.dma("sync", gfin[:], I["finalgT"][:, :], (), ["gfin"])
        for j in range(0, D, 512):
            P.dma("pool", xs[j:j + 512, :], I["xT"][j:j + 512, :], (), [f"xs{j}"])
        cond = P.sb("cond", [128, KC * 2], F32)
        sil = P.sb("sil", [128, KC * 2], F32)
        adab = P.sb("adab", [128, DEPTH * 96], F32)
        P.dma("sync", cond[:], I["condT"][:, :], (), ["cond"])
        P.dma("sync", adab[:], I["ada_bT"][:, :], (), ["adab"])
        P.act(sil[:], cond[:], AF.Silu, ["cond"], ["sil"])
        GW = 512
        wst = [P.sb(f"adaw{i}", [128, KC, GW], F32) for i in range(2)]
        wbf_ = [P.sb(f"adab{i}", [128, KC, GW], BF16) for i in range(2)]
        silb = P.sb("silb", [128, KC * 2], BF16)
        P.copy("dve", silb[:], sil[:], ["sil"], ["silb"])
        aps = P.ps("adaps", [128, 4, 512], F32)
        ngrp = 6 * D // GW
        glist = [(l, cg) for l in range(DEPTH) for cg in range(ngrp)]

        def aload(gi):
            l, cg = glist[gi]
            b = gi % 2
            src = I["ada_w"][l, :, cg * GW:(cg + 1) * GW].rearrange("(k p) n -> p k n", p=128)
            P.dma("sync" if b == 0 else "pool", wst[b][:], src, (), [f"adaw{b}"])
            P.copy("act" if b == 0 else "dve", wbf_[b][:], wst[b][:], [f"adaw{b}"], [f"adab{b}"])
        aload(0)
        for gi, (l, cg) in enumerate(glist):
            b = gi % 2
            if gi + 1 < len(glist):
                aload(gi + 1)
            for mb in range(GW // 128):
                m = cg * (GW // 128) + mb
                slot = m % 4
                for k in range(KC):
                    P.mm(aps[:, slot, 0:2], wbf_[b][:, k, mb * 128:(mb + 1) * 128], silb[:, 2 * k:2 * k + 2],
                         k == 0, k == KC - 1, [f"adab{b}", "silb"], [f"adaps{slot}"], lazy=True)
                idx = (l * 96 + m) * 2
                P.ts("dve", modT[:, idx:idx + 2], aps[:, slot, 0:2], adab[:, l * 96 + m:l * 96 + m + 1], None,
                     ALU.add, None, [f"adaps{slot}", "adab"], ["modT"])

    def norm_phase(l, which, hT, tiles, gtile, router=None):
        xt = [P.sb(f"nx{i}", [128, KC, 512], F32) for i in range(2)]
        sq = [P.sb(f"nsq{i}", [128, 512], F32) for i in range(2)]
        rb = [P.sb(f"nrb{i}", [128, 512], F32) for i in range(2)]
        tmp = [P.sb(f"ntmp{i}", [128, 512], F32) for i in range(3)]
        Aco = P.sb("nA", [128, KC * 2], F32)
        nps = P.ps("nps", [128, 2, 512], F32)
        vs, vc = (0, 1) if which == 0 else (3, 4)
        for k in range(KC):
            for c in range(2):
                P.stt(Aco[:, 2 * k + c:2 * k + c + 1], mod(l, vc, k, c), 1.0, gtile[:, l * KC + k:l * KC + k + 1],
                      ALU.add, ALU.mult, ["modT", "gmix", "gffn"], ["nA"])
        if router is not None:
            h32 = P.sb("h32", [128, KC, 512], F32)
            rw = P.sb("rw", [128, KC, E], F32)
            rbias = P.sb("rbias", [1, E], F32)
            rps = P.ps("rps", [128, 2, 512], F32)
            P.dma("sync", rw[:], I["router_w"][l].rearrange("(k p) e -> p k e", p=128), (), ["rw"])
            P.dma("sync", rbias[:], I["router_b"][0:1, l * E:(l + 1) * E], (), ["rbias"])
        for ti, (t0, n) in enumerate(tiles):
            c = 0 if t0 >= L else 1
            b = ti % 2
            P.dma("sync", xt[b][:, :, :n], xs[:, t0:t0 + n].rearrange("(k p) t -> p k t", p=128),
                  [f"xs{j}" for j in range(0, D, 512)], [f"nx{b}"])
            for k in range(KC):
                s = sq[k % 2]
                P.act(s[:, :n], xt[b][:, k, :n], AF.Square, [f"nx{b}"], [f"nsq{k % 2}"])
                P.mm(nps[:, b, :n], ones_f[:], s[:, :n], k == 0, k == KC - 1, ["ones_f", f"nsq{k % 2}"], [f"nps{b}"])
            P.ts("dve", rb[b][:, :n], nps[:, b, :n], 1.0 / D, cfg.EPS, ALU.mult, ALU.add, [f"nps{b}"], [f"nrb{b}"])
            P.act(rb[b][:, :n], rb[b][:, :n], AF.Sqrt, [f"nrb{b}"], [f"nrb{b}"])
            P.emit("dve", lambda e, a=rb[b][:, :n]: e.reciprocal(out=a, in_=a), [f"nrb{b}"], [f"nrb{b}"])
            for k in range(KC):
                tb = tmp[k % 3]
                P.tt("dve" if k % 2 == 0 else "pool", tb[:, :n], xt[b][:, k, :n], rb[b][:, :n], ALU.mult,
                     [f"nx{b}", f"nrb{b}"], [f"ntmp{k % 3}"])
                if router is not None:
                    P.act(h32[:, k, :n], tb[:, :n], AF.Identity, [f"ntmp{k % 3}", "nA", "modT"], ["h32"],
                          bias=mod(l, vs, k, c), scale=Aco[:, 2 * k + c:2 * k + c + 1])
                    P.copy("pool", hT[:, k, t0:t0 + n], h32[:, k, :n], ["h32"], [f"hT{ti}"])
                else:
                    P.act(hT[:, k, t0:t0 + n], tb[:, :n], AF.Identity, [f"ntmp{k % 3}", "nA", "modT"], [f"hT{ti}"],
                          bias=mod(l, vs, k, c), scale=Aco[:, 2 * k + c:2 * k + c + 1])
            if router is not None:
                router["tile"](ti, t0, n, h32, rw, rbias, rps)

    def linear(name, actT, act_reads, KCin, W, col0, ncols, tiles, evac, GW=256, mode="F"):
        wst = [P.sb(f"{name}_st{i}", [128, KCin, GW], F32) for i in range(2)]
        wbf = [P.sb(f"{name}_bf{i}", [128, KCin, GW], BF16) for i in range(2)]
        lps = P.ps(f"{name}_ps", [128, 4, 512], F32)
        pi = 0
        ngr = ncols // GW

        def load(cg):
            b = cg % 2
            src = W[:, col0 + cg * GW:col0 + (cg + 1) * GW].rearrange("(k p) n -> p k n", p=128)
            P.dma("sync", wst[b][:], src, (), [f"{name}_st{b}"])
            P.copy("act", wbf[b][:], wst[b][:], [f"{name}_st{b}"], [f"{name}_bf{b}"])
        load(0)
        for cg in range(ngr):
            b = cg % 2
            if cg + 1 < ngr:
                load(cg + 1)
            if mode == "F":
                for mb in range(GW // 128):
                    for (t0, n) in tiles:
                        slot = pi % 4
                        pi += 1
                        for k in range(KCin):
                            P.mm(lps[:, slot, :n], wbf[b][:, k, mb * 128:(mb + 1) * 128], actT[:, k, t0:t0 + n],
                                 k == 0, k == KCin - 1, [f"{name}_bf{b}"] + act_reads, [f"{name}_ps{slot}"], lazy=True)
                        evac(lps[:, slot, :n], f"{name}_ps{slot}", (col0 + cg * GW) // 128 + mb, t0, n)
            else:
                for (t0, n) in tiles:
                    for tc in range(t0, t0 + n, 128):
                        slot = pi % 4
                        pi += 1
                        for k in range(KCin):
                            P.mm(lps[:, slot, :GW], actT[:, k, tc:tc + 128], wbf[b][:, k, :],
                                 k == 0, k == KCin - 1, [f"{name}_bf{b}"] + act_reads, [f"{name}_ps{slot}"], lazy=True)
                        evac(lps[:, slot, :GW], f"{name}_ps{slot}", cg, tc)

    XS_ALL = [f"xs{j}" for j in range(0, D, 512)]

    def residual_evac(l, gate_v, tag):
        xo = [P.sb(f"{tag}_xo{i}", [128, 512], F32) for i in range(3)]
        st = {"i": 0}

        def ev(ps_ap, psname, m, t0, n):
            i = st["i"] % 3
            st["i"] += 1
            c = 0 if t0 >= L else 1
            xname = f"xs{(m * 128) // 512 * 512}"
            P.dma("act", xo[i][:, :n], xs[m * 128:(m + 1) * 128, t0:t0 + n], [xname], [f"{tag}_xo{i}"])
            P.stt(xo[i][:, :n], ps_ap, mod(l, gate_v, m, c), xo[i][:, :n], ALU.mult, ALU.add,
                  [psname, "modT", f"{tag}_xo{i}"], [f"{tag}_xo{i}"])
            P.dma("pool", xs[m * 128:(m + 1) * 128, t0:t0 + n], xo[i][:, :n], [f"{tag}_xo{i}"], [xname])
        return ev

    def even_mixer(l):
        i = l // 2
        last = (l == DEPTH - 1)
        all_tiles = tok_tiles(0, T, L)
        out_tiles = tok_tiles(L if last else 0, T, L)
        with P.phase():
            hT = P.sb("hT", [128, KC, T], BF16)
            with P.phase():
                norm_phase(l, 0, hT, all_tiles, gmix)
            hreads = [f"hT{ti}" for ti in range(len(all_tiles))]
            stg = [P.sb(f"ustg{j}", [128, 512], F32) for j in range(3)]
            st = {"i": 0}

            def ev(ps_ap, psname, m, t0, n):
                j = st["i"] % 3
                st["i"] += 1
                P.copy("act" if st["i"] % 2 else "dve", stg[j][:, :n], ps_ap, [psname], [f"ustg{j}"])
                P.dma("pool", uT_d[m * 128:(m + 1) * 128, t0:t0 + n], stg[j][:, :n], [f"ustg{j}"], [f"uT{m}"])
            linear("win", hT, hreads, KC, I["ev_w_in"][i], 0, D, all_tiles, ev)
        if STAGE == "E2":
            raise StopBuild()
        with P.phase():
            NPD = 2 * NPAIR
            base = i * NPD
            lre = P.sb("lre", [128, NPD], F32)
            lim = P.sb("lim", [128, NPD], F32)
            ldt = P.sb("ldt", [128, NPD], F32)
            P.dma("sync", lre[:], I["lamreT"][:, base:base + NPD], (), ["lre"])
            P.dma("sync", lim[:], I["lamimT"][:, base:base + NPD], (), ["lim"])
            P.dma("sync", ldt[:], I["logdtT"][:, base:base + NPD], (), ["ldt"])
            dt = P.sb("dt", [128, NPD], F32)
            th = P.sb("th", [128, NPD], F32)
            rr = P.sb("rr", [128, NPD], F32)
            cs = P.sb("cs", [128, NPD], F32)
            sn = P.sb("sn", [128, NPD], F32)
            t1 = P.sb("t1", [128, NPD], F32)
            t2 = P.sb("t2", [128, NPD], F32)
            NST = max(1, math.ceil(math.log2(T)))
            pre = P.sb("pre", [128, NST, NPD], F32)
            pim = P.sb("pim", [128, NST, NPD], F32)
            pimn = P.sb("pimn", [128, NST, NPD], F32)
            cre = P.sb("cre", [128, NPD], F32)
            cim = P.sb("cim", [128, NPD], F32)
            cimn = P.sb("cimn", [128, NPD], F32)
            PR = ["lre", "lim", "ldt", "dt", "th", "rr", "cs", "sn", "t1", "t2", "pre", "pim", "pimn", "cre", "cim", "cimn"]
            pi_ = math.pi
            P.act(dt[:], ldt[:], AF.Exp, PR, PR)
            P.tt("dve", rr[:], dt[:], lre[:], ALU.mult, PR, PR)
            P.act(rr[:], rr[:], AF.Exp, PR, PR)
            P.tt("dve", th[:], dt[:], lim[:], ALU.mult, PR, PR)
            ki = P.sb("ki", [128, NPD], mybir.dt.int32)
            for (dst, off) in ((sn, 0.0), (cs, 0.25)):
                P.ts("dve", t1[:], th[:], 1.0 / (2 * pi_), 8.0 + off, ALU.mult, ALU.add, PR, PR)
                P.copy("dve", ki[:], t1[:], PR, PR + ["ki"])
                P.copy("dve", t2[:], ki[:], PR + ["ki"], PR)
                P.tt("dve", t1[:], t1[:], t2[:], ALU.subtract, PR, PR)
                P.ts("dve", t2[:], t1[:], 0.5, None, ALU.is_gt, None, PR, PR)
                P.tt("dve", t1[:], t1[:], t2[:], ALU.subtract, PR, PR)
                P.ts("dve", t2[:], t1[:], -0.5, None, ALU.is_lt, None, PR, PR)
                P.tt("dve", t1[:], t1[:], t2[:], ALU.add, PR, PR)
                P.act(dst[:], t1[:], AF.Sin, PR, PR, scale=2 * pi_)
            P.tt("dve", pre[:, 0, :], rr[:], cs[:], ALU.mult, PR, PR)
            P.tt("dve", pim[:, 0, :], rr[:], sn[:], ALU.mult, PR, PR)
            for k in range(1, NST):
                P.tt("dve", t1[:], pre[:, k - 1, :], pre[:, k - 1, :], ALU.mult, PR, PR)
                P.tt("dve", t2[:], pim[:, k - 1, :], pim[:, k - 1, :], ALU.mult, PR, PR)
                P.tt("dve", pre[:, k, :], t1[:], t2[:], ALU.subtract, PR, PR)
                P.tt("dve", t1[:], pre[:, k - 1, :], pim[:, k - 1, :], ALU.mult, PR, PR)
                P.ts("dve", pim[:, k, :], t1[:], 2.0, None, ALU.mult, None, PR, PR)
            P.ts("dve", pimn[:], pim[:], -1.0, None, ALU.mult, None, PR, PR)
            P.ts("dve", t1[:], pre[:, 0, :], -1.0, None, ALU.add, None, PR, PR)
            P.tt("dve", cre[:], t1[:], lre[:], ALU.mult, PR, PR)
            P.tt("dve", t2[:], pim[:, 0, :], lim[:], ALU.mult, PR, PR)
            P.tt("dve", cre[:], cre[:], t2[:], ALU.add, PR, PR)
            P.tt("dve", cim[:], pim[:, 0, :], lre[:], ALU.mult, PR, PR)
            P.tt("dve", t2[:], t1[:], lim[:], ALU.mult, PR, PR)
            P.tt("dve", cim[:], cim[:], t2[:], ALU.subtract, PR, PR)
            P.tt("dve", t1[:], lre[:], lre[:], ALU.mult, PR, PR)
            P.tt("dve", t2[:], lim[:], lim[:], ALU.mult, PR, PR)
            P.tt("dve", t1[:], t1[:], t2[:], ALU.add, PR, PR)
            P.emit("dve", lambda e: e.reciprocal(out=t1[:], in_=t1[:]), PR, PR)
            P.tt("dve", cre[:], cre[:], t1[:], ALU.mult, PR, PR)
            P.tt("dve", cim[:], cim[:], t1[:], ALU.mult, PR, PR)
            P.ts("dve", cimn[:], cim[:], -1.0, None, ALU.mult, None, PR, PR)

            uch = [P.sb(f"uch{j}", [128, T], F32) for j in range(2)]
            Z = [[P.sb(f"Z{p_}{d}", [128, 2, T], F32) for d in range(2)] for p_ in range(2)]
            CB = 256 if T % 256 == 0 else 128
            pwt = [P.sb(f"pwt{d}", [128, 4, CB], F32) for d in range(4)]
            bw = [P.sb(f"bw{j}", [128, 2, 128], F32) for j in range(4)]
            cw = [P.sb(f"cw{j}", [128, 2, 32], F32) for j in range(4)]
            bps = P.ps("bps", [128, 2, 2, 512], F32)
            yps = P.ps("yps", [32, 2, 2, 512], F32)
            ysb = [P.sb(f"ysb{j}", [32, 512], F32) for j in range(2)]
            ysb2 = [P.sb(f"ysc{j}", [32, 512], F32) for j in range(2)]

            def zcol(d, t0):
                if d == 0:
                    return t0
                return t0 - L if t0 >= L else S + t0

            yi = 0
            for gp0 in range(0, NPAIR, 2):
              allres = {}
              chains = []
              for gp in (gp0, gp0 + 1):
                pi2 = gp % 2
                kc = gp // 4
                ub = kc % 2
                if gp % 4 == 0:
                    P.dma("sync", uch[ub][:], uT_d[kc * 128:(kc + 1) * 128, :], [f"uT{kc}"], [f"uch{ub}"])
                res = []
                allres[gp] = res
                for d in range(2):
                    col = d * NPAIR + gp
                    wb = (gp * 2 + d) % 4
                    eng = "dve"
                    P.dma("act", bw[wb][:, 0, :], I["bblk_re"][base + col], (), [f"bw{wb}"])
                    P.dma("act", bw[wb][:, 1, :], I["bblk_im"][base + col], (), [f"bw{wb}"])
                    P.dma("act", cw[wb][:, 0, :], I["cblk_re"][base + col], (), [f"cw{wb}"])
                    P.dma("act", cw[wb][:, 1, :], I["cblk_im"][base + col], (), [f"cw{wb}"])
                    A = Z[pi2][d]
                    an = f"Z{pi2}{d}"
                    for (t0, n) in all_tiles:
                        z0 = zcol(d, t0)
                        for ri in range(2):
                            P.mm(bps[:, d, ri, :n], bw[wb][:, ri, :], uch[ub][:, t0:t0 + n], True, True,
                                 [f"bw{wb}", f"uch{ub}"], [f"bps{d}{ri}"])
                        P.ts("dve", A[:, 0, z0:z0 + n], bps[:, d, 0, :n], cre[:, col:col + 1], None, ALU.mult, None,
                             [f"bps{d}0", "cre"], [an])
                        P.stt(A[:, 0, z0:z0 + n], bps[:, d, 1, :n], cimn[:, col:col + 1], A[:, 0, z0:z0 + n],
                              ALU.mult, ALU.add, [f"bps{d}1", "cimn", an], [an])
                        P.ts("dve", A[:, 1, z0:z0 + n], bps[:, d, 1, :n], cre[:, col:col + 1], None, ALU.mult, None,
                             [f"bps{d}1", "cre"], [an])
                        P.stt(A[:, 1, z0:z0 + n], bps[:, d, 0, :n], cim[:, col:col + 1], A[:, 1, z0:z0 + n],
                              ALU.mult, ALU.add, [f"bps{d}0", "cim", an], [an])
                    res.append((A, an, wb))
                for d in range(2):
                    col = d * NPAIR + gp
                    X, xn = Z[pi2][d], f"Z{pi2}{d}"
                    pw, pn = pwt[pi2 * 2 + d], f"pwt{pi2 * 2 + d}"
                    ops = []
                    chains.append(ops)
                    Xv = [X[:, pl, :].rearrange("p (b c) -> p b c", c=CB) for pl in range(2)]

                    def cupd(dst_re, dst_im, src_re, src_im, kk, rd, wr, ops=ops, col=col):
                        ar = pre[:, kk, col:col + 1]
                        ai = pim[:, kk, col:col + 1]
                        ain = pimn[:, kk, col:col + 1]
                        ops.append((dst_re, src_re, ar, dst_re, rd, wr))
                        ops.append((dst_re, src_im, ain, dst_re, rd, wr))
                        ops.append((dst_im, src_im, ar, dst_im, rd, wr))
                        ops.append((dst_im, src_re, ai, dst_im, rd, wr))
                    if d == 0:
                        i0 = 0
                    else:
                        i0 = CB - 1
                    ops.append(("copy", pw[:, 0, i0:i0 + 1], pre[:, 0, col:col + 1], [pn, "pre"], [pn]))
                    ops.append(("copy", pw[:, 1, i0:i0 + 1], pim[:, 0, col:col + 1], [pn, "pim"], [pn]))
                    sz = 1
                    kk = 0
                    while sz < CB:
                        if d == 0:
                            srcs, dsts = slice(0, sz), slice(sz, 2 * sz)
                        else:
                            srcs, dsts = slice(CB - sz, CB), slice(CB - 2 * sz, CB - sz)
                        ar = pre[:, kk, col:col + 1]
                        ai = pim[:, kk, col:col + 1]
                        ain = pimn[:, kk, col:col + 1]
                        ops.append(("ts", pw[:, 0, dsts], pw[:, 0, srcs], ar, [pn, "pre"], [pn]))
                        ops.append((pw[:, 0, dsts], pw[:, 1, srcs], ain, pw[:, 0, dsts], [pn, "pimn"], [pn]))
                        ops.append(("ts", pw[:, 1, dsts], pw[:, 1, srcs], ar, [pn, "pre"], [pn]))
                        ops.append((pw[:, 1, dsts], pw[:, 0, srcs], ai, pw[:, 1, dsts], [pn, "pim"], [pn]))
                        sz *= 2
                        kk += 1
                    ops.append(("ts", pw[:, 2, :], pw[:, 1, :], -1.0, [pn], [pn]))
                    ops.append(("copy", pw[:, 3, :], pw[:, 0, :], [pn], [pn]))
                    LOGC = CB.bit_length() - 1
                    for kk in range(LOGC):
                        s_ = 1 << kk
                        if d == 0:
                            dpos, spos = slice(2 * s_ - 1, CB, 2 * s_), slice(s_ - 1, CB, 2 * s_)
                        else:
                            dpos, spos = slice(0, CB, 2 * s_), slice(s_, CB, 2 * s_)
                        cupd(Xv[0][:, :, dpos], Xv[1][:, :, dpos], Xv[0][:, :, spos], Xv[1][:, :, spos], kk,
                             [xn, "pre", "pim", "pimn"], [xn])
                    for kk in range(LOGC - 2, -1, -1):
                        s_ = 1 << kk
                        if d == 0:
                            dpos, spos = slice(3 * s_ - 1, CB, 2 * s_), slice(2 * s_ - 1, CB - s_, 2 * s_)
                        else:
                            dpos, spos = slice(s_, CB - 2 * s_, 2 * s_), slice(2 * s_, CB, 2 * s_)
                        cupd(Xv[0][:, :, dpos], Xv[1][:, :, dpos], Xv[0][:, :, spos], Xv[1][:, :, spos], kk,
                             [xn, "pre", "pim", "pimn"], [xn])
                    NB = T // CB
                    order = range(1, NB) if d == 0 else range(NB - 2, -1, -1)
                    for j in order:
                        if d == 0:
                            cpos = (j - 1) * CB + CB - 1
                        else:
                            cpos = (j + 1) * CB
                        c_re = X[:, 0, cpos:cpos + 1]
                        c_im = X[:, 1, cpos:cpos + 1]
                        blk = slice(j * CB, (j + 1) * CB)
                        rd, wr = [xn, pn], [xn]
                        ops.append((X[:, 0:2, blk], pw[:, 0:2, :], c_re, X[:, 0:2, blk], rd, wr))
                        ops.append((X[:, 0:2, blk], pw[:, 2:4, :], c_im, X[:, 0:2, blk], rd, wr))
              for oi in range(max(len(c_) for c_ in chains)):
                    for d in range(len(chains)):
                        if oi >= len(chains[d]):
                            continue
                        op = chains[d][oi]
                        if op[0] == "copy":
                            P.copy("dve", op[1], op[2], op[3], op[4])
                        elif op[0] == "ts":
                            P.ts("dve", op[1], op[2], op[3], None, ALU.mult, None, op[4], op[5])
                        else:
                            out_, in0_, sc_, in1_, rd, wr = op
                            P.stt(out_, in0_, sc_, in1_, ALU.mult, ALU.add, rd, wr)
              for gp in (gp0, gp0 + 1):
                res = allres[gp]
                for (t0, n) in all_tiles:
                    yb = yi % 2
                    yi += 1
                    for ri in range(2):
                        for d in range(2):
                            zt, zn, wb = res[d]
                            z0 = zcol(d, t0)
                            P.mm(yps[:, yb, ri, :n], cw[wb][:, ri, :], zt[:, ri, z0:z0 + n], d == 0, d == 1,
                                 [f"cw{wb}", zn], [f"yps{yb}{ri}"])
                    P.copy("act", ysb[yb][:, :n], yps[:, yb, 1, :n], [f"yps{yb}1"], [f"ysb{yb}"])
                    P.tt("dve", ysb2[yb][:, :n], yps[:, yb, 0, :n], ysb[yb][:, :n], ALU.subtract,
                         [f"yps{yb}0", f"ysb{yb}"], [f"ysc{yb}"])
                    P.dma("pool", yT_d[gp * 32:(gp + 1) * 32, t0:t0 + n], ysb2[yb][:, :n], [f"ysc{yb}"], [f"yT{gp // 4}"])
        if STAGE == "E3":
            raise StopBuild()
        with P.phase():
            catT = P.sb("catT", [128, KC, T], BF16)
            with P.phase():
                gT = P.sb("gT", [128, SC, T], BF16)
                sdT = P.sb("sdT", [128, SC], F32)
                glb = P.sb("glb", [128, SC], F32)
                P.dma("sync", sdT[:], I["s5dT"][:, i * SC:(i + 1) * SC], (), ["sdT"])
                P.dma("sync", glb[:], I["glu_bT"][:, i * SC:(i + 1) * SC], (), ["glb"])
                with P.phase():
                    yb_ = [P.sb(f"ey{j}", [128, T], F32) for j in range(2)]
                    ub_ = [P.sb(f"eu{j}", [128, T], F32) for j in range(2)]
                    w1 = [P.sb(f"ew{j}", [128, T], F32) for j in range(2)]
                    for m in range(SC):
                        b = m % 2
                        y = yb_[b]
                        P.dma("sync", y[:], yT_d[m * 128:(m + 1) * 128, :], (), [f"ey{b}"])
                        P.dma("act", ub_[b][:], uT_d[m * 128:(m + 1) * 128, :], (), [f"eu{b}"])
                        P.stt(y[:], ub_[b][:], sdT[:, m:m + 1], y[:], ALU.mult, ALU.add, [f"eu{b}", "sdT", f"ey{b}"], [f"ey{b}"])
                        P.tt("pool", w1[b][:], y[:], y[:], ALU.mult, [f"ey{b}"], [f"ew{b}"])
                        P.ts("pool", w1[b][:], w1[b][:], 0.044715, 1.0, ALU.mult, ALU.add, [f"ew{b}"], [f"ew{b}"])
                        P.tt("pool", w1[b][:], w1[b][:], y[:], ALU.mult, [f"ew{b}", f"ey{b}"], [f"ew{b}"])
                        P.act(w1[b][:], w1[b][:], AF.Sigmoid, [f"ew{b}"], [f"ew{b}"], scale=2.0 * math.sqrt(2.0 / math.pi))
                        P.tt("dve", gT[:, m, :], y[:], w1[b][:], ALU.mult, [f"ey{b}", f"ew{b}"], ["gT"])
                    if STAGE == "E4":
                        raise StopBuild()
                with P.phase():
                    sg = [P.sb(f"sg{j}", [128, 512], BF16) for j in range(3)]
                    st = {"i": 0}

                    def ev_glu(ps_ap, psname, m, t0, n):
                        j = st["i"] % 3
                        st["i"] += 1
                        P.act(sg[j][:, :n], ps_ap, AF.Sigmoid, [psname, "glb"], [f"sg{j}"], bias=glb[:, m:m + 1])
                        P.tt("dve", catT[:, m, t0:t0 + n], gT[:, m, t0:t0 + n], sg[j][:, :n], ALU.mult,
                             ["gT", f"sg{j}"], [f"cat{m}"])
                    linear("glu", gT, ["gT"], SC, I["glu_w"][i], 0, cfg.S5W, all_tiles, ev_glu)
                    if STAGE == "E5":
                        raise StopBuild()
            with P.phase():
                dd_ = P.sb("dftd", [128, 256], F32)
                P.dma("sync", dd_[:], I["dft_d"][:, :], (), ["dftd"])
                fw = P.sb("fw", [128, 4, 128], F32)
                fwb = P.sb("fwb", [128, 4, 128], BF16)
                P.dma("sync", fw[:], I["fnet_w"][i * 4:(i + 1) * 4].rearrange("h d e -> d h e"), (), ["fw"])
                P.copy("dve", fwb[:], fw[:], ["fw"], ["fwb"])
                fch = [P.sb(f"fch{j}", [128, T], F32) for j in range(2)]
                xcs = [P.sb(f"xcs{j}", [128, TCH, 256], F32) for j in range(2)]
                fps = P.ps("fps", [128, 2, 512], F32)
                TW = 256
                tabc = [P.sb(f"tabc{j}", [128, max(S, L) // 128, TW], F32) for j in range(2)]
                tabs = [P.sb(f"tabs{j}", [128, max(S, L) // 128, TW], F32) for j in range(2)]
                sps = P.ps("sps", [128, 2, 512], F32)
                ops_ = P.ps("ops", [128, 2, 512], F32)
                spb = [P.sb(f"spb{j}", [128, TW], BF16) for j in range(2)]
                segs = [(0, L, I["dftc_ctx"], I["dfts_ctx"]), (L, S, I["dftc_lat"], I["dfts_lat"])]
                ti = 0
                for h in range(4):
                    b = h % 2
                    P.dma("sync", fch[b][:], uT_d[(SC + h) * 128:(SC + h + 1) * 128, :], (), [f"fch{b}"])
                    for ch in range(TCH):
                        s_ = ch % 2
                        P.mm(fps[:, s_, :256], fch[b][:, ch * 128:(ch + 1) * 128], dd_[:], True, True,
                             [f"fch{b}", "dftd"], [f"fps{s_}"])
                        P.copy("act" if ch % 2 else "dve", xcs[b][:, ch, :], fps[:, s_, :256], [f"fps{s_}"], [f"xcs{b}"])
                    for (seg0, seglen, tc_, tsn_) in segs:
                        if STAGE == "F1":
                            break
                        nch = seglen // 128
                        ch0 = seg0 // 128
                        for c0 in range(0, seglen, TW):
                            w = min(TW, seglen - c0)
                            tb = ti % 2
                            ti += 1
                            P.dma("sync", tabc[tb][:, :nch, :w], tc_[:, c0:c0 + w].rearrange("(k p) t -> p k t", p=128),
                                  (), [f"tabc{tb}"])
                            P.dma("act", tabs[tb][:, :nch, :w], tsn_[:, c0:c0 + w].rearrange("(k p) t -> p k t", p=128),
                                  (), [f"tabs{tb}"])
                            for ch in range(nch):
                                P.mm(sps[:, tb, :w], xcs[b][:, ch0 + ch, 0:128], tabc[tb][:, ch, :w], ch == 0, False,
                                     [f"xcs{b}", f"tabc{tb}"], [f"sps{tb}"])
                                P.mm(sps[:, tb, :w], xcs[b][:, ch0 + ch, 128:256], tabs[tb][:, ch, :w], False, ch == nch - 1,
                                     [f"xcs{b}", f"tabs{tb}"], [f"sps{tb}"])
                            P.copy("act", spb[tb][:, :w], sps[:, tb, :w], [f"sps{tb}"], [f"spb{tb}"])
                            if STAGE == "F2":
                                continue
                            P.mm(ops_[:, tb, :w], fwb[:, h, :], spb[tb][:, :w], True, True, ["fwb", f"spb{tb}"], [f"ops{tb}"])
                            P.copy("dve", catT[:, SC + h, seg0 + c0:seg0 + c0 + w], ops_[:, tb, :w], [f"ops{tb}"],
                                   [f"cat{SC + h}"])
            if STAGE in ("E5F", "F1", "F2"):
                raise StopBuild()
            with P.phase():
                ev = residual_evac(l, 2, "eo")
                linear("wout", catT, [f"cat{m}" for m in range(KC)], KC, I["ev_w_out"][i], 0, D, out_tiles, ev)
        return out_tiles

    def odd_mixer(l):
        i = l // 2
        last = (l == DEPTH - 1)
        all_tiles = tok_tiles(0, T, L)
        out_tiles = tok_tiles(L if last else 0, T, L)
        KVO = 4 * 128 + cfg.NQH * 128
        VO = KVO + cfg.NKV * 128
        scale = cfg.HD ** -0.5
        with P.phase():
            hT = P.sb("hT", [128, KC, T], BF16)
            with P.phase():
                norm_phase(l, 0, hT, all_tiles, gmix)
            hreads = [f"hT{ti}" for ti in range(len(all_tiles))]
            with P.phase():
                stg = [P.sb(f"ostg{j}", [128, 512], F32) for j in range(3)]
                st = {"i": 0}

                def ev(ps_ap, psname, m, t0, n):
                    j = st["i"] % 3
                    st["i"] += 1
                    P.copy("act" if st["i"] % 2 else "dve", stg[j][:, :n], ps_ap, [psname], [f"ostg{j}"])
                    P.dma("pool", pT_d[m * 128:(m + 1) * 128, t0:t0 + n], stg[j][:, :n], [f"ostg{j}"], [f"pT{m}"])
                linear("oin", hT, hreads, KC, I["od_w_in"][i], 0, VO, all_tiles, ev)
            with P.phase():
                vst = [P.sb(f"vstg{j}", [128, 256], BF16) for j in range(3)]
                st2 = {"i": 0}

                def evv(ps_ap, psname, cg, tc):
                    j = st2["i"] % 3
                    st2["i"] += 1
                    P.copy("act" if st2["i"] % 2 else "dve", vst[j][:], ps_ap, [psname], [f"vstg{j}"])
                    P.dma("pool", vtok_d[tc:tc + 128, cg * 256:(cg + 1) * 256], vst[j][:], [f"vstg{j}"], ["vtok_d"])
                linear("vin", hT, hreads, KC, I["od_w_in"][i], VO, 512, all_tiles, evv, mode="T")
        with P.phase():
            catT = P.sb("catT", [128, KC, T], BF16)
            with P.phase():
                PADW = 16
                pw_f = P.sb("pw_f", [128, 4, 128], F32)
                pw_b = P.sb("pw_b", [128, 4, 128], BF16)
                psc = P.sb("psc", [128, 4], F32)
                P.dma("sync", pw_f[:], I["pool_w"][i * 4:(i + 1) * 4].rearrange("g d e -> d g e"), (), ["pw_f"])
                P.copy("dve", pw_b[:], pw_f[:], ["pw_f"], ["pw_b"])
                P.dma("sync", psc[:], I["pool_scaleT"][:, i * 4:(i + 1) * 4], (), ["psc"])
                W_ = max(S, L) + 2 * PADW
                xb = [P.sb(f"pxb{j}", [128, W_], F32) for j in range(2)]
                ya = [P.sb(f"pya{j}", [128, W_], F32) for j in range(2)]
                rc = P.sb("prc", [128, max(S, L)], F32)
                pb = [P.sb(f"ppb{j}", [128, max(S, L)], BF16) for j in range(2)]
                pps = P.ps("pps", [128, 2, 512], F32)
                ci = 0
                for gi, w in enumerate(cfg.POOL_WINDOWS):
                    for (seg0, seglen, rcrow) in ((0, L, 0), (L, S, 1)):
                        b = ci % 2
                        ci += 1
                        X = xb[b]
                        P.memset("pool", X[:, 0:PADW], 0.0, [f"pxb{b}"])
                        P.memset("pool", X[:, PADW + seglen:2 * PADW + seglen], 0.0, [f"pxb{b}"])
                        P.dma("sync", X[:, PADW:PADW + seglen], pT_d[gi * 128:(gi + 1) * 128, seg0:seg0 + seglen], (), [f"pxb{b}"])
                        P.dma("act", rc[:, :seglen], I["pool_rc"][gi * 2 + rcrow:gi * 2 + rcrow + 1, :seglen].partition_broadcast(128),
                              (), ["prc"])
                        WW = seglen + 2 * PADW
                        cur, curname = X, f"pxb{b}"
                        bufs = [(ya[0], "pya0"), (ya[1], "pya1")]
                        sh = 1
                        step = 0
                        while sh < w:
                            dst, dname = bufs[step % 2]
                            if sh == 1:
                                P.memset("pool", dst[:, 0:1], 0.0, [dname])
                                P.tt("dve", dst[:, 1:WW], cur[:, 0:WW - 1], cur[:, 1:WW], ALU.add, [curname], [dname])
                            else:
                                h2 = sh // 2
                                P.memset("pool", dst[:, 0:h2], 0.0, [dname])
                                P.memset("pool", dst[:, WW - h2:WW], 0.0, [dname])
                                P.tt("dve", dst[:, h2:WW - h2], cur[:, 0:WW - 2 * h2], cur[:, 2 * h2:WW], ALU.add,
                                     [curname], [dname])
                            cur, curname = dst, dname
                            sh *= 2
                            step += 1
                        dst, dname = bufs[step % 2]
                        P.tt("dve", dst[:, :seglen], cur[:, PADW:PADW + seglen], rc[:, :seglen], ALU.mult,
                             [curname, "prc"], [dname])
                        P.tt("dve", pb[b][:, :seglen], dst[:, :seglen], X[:, PADW:PADW + seglen], ALU.subtract,
                             [dname, f"pxb{b}"], [f"ppb{b}"])
                        for (t0, n) in tok_tiles(seg0, seg0 + seglen, L):
                            q = (t0 // 512) % 2
                            P.mm(pps[:, q, :n], pw_b[:, gi, :], pb[b][:, t0 - seg0:t0 - seg0 + n], True, True,
                                 ["pw_b", f"ppb{b}"], [f"pps{q}"])
                            P.act(catT[:, gi, t0:t0 + n], pps[:, q, :n], AF.Identity, [f"pps{q}", "psc"], [f"cat{gi}"],
                                  scale=psc[:, gi:gi + 1])
            with P.phase():
                qT = P.sb("qT", [128, cfg.NQH, T], BF16)
                kT = P.sb("kT", [128, cfg.NKV, T], BF16)
                vtok = P.sb("vtok", [128, TCH, 512], BF16)
                P.dma("sync", vtok[:], vtok_d[:, :].rearrange("(c p) d -> p c d", p=128), (), ["vtok"])
                with P.phase():
                    rR = P.sb("rR", [128, 128], F32)
                    qg_all = P.sb("qg_all", [128, 2, NO], F32)
                    P.dma("sync", rR[:], I["ropeR"][:, :], (), ["rR"])
                    P.dma("sync", qg_all[:, 0, :], I["qgainT"][:, :], (), ["qg"])
                    P.dma("sync", qg_all[:, 1, :], I["kgainT"][:, :], (), ["qg"])
                    raw = [P.sb(f"qraw{j}", [128, 512], F32) for j in range(2)]
                    rC = [P.sb(f"rC{j}", [128, 512], F32) for j in range(2)]
                    rS = [P.sb(f"rS{j}", [128, 512], F32) for j in range(2)]
                    sq = [P.sb(f"qsq{j}", [128, 512], F32) for j in range(2)]
                    rb = [P.sb(f"qrb{j}", [128, 512], F32) for j in range(2)]
                    qn = [P.sb(f"qn{j}", [128, 512], F32) for j in range(2)]
                    t1 = [P.sb(f"qt1{j}", [128, 512], F32) for j in range(2)]
                    t2 = [P.sb(f"qt2{j}", [128, 512], F32) for j in range(2)]
                    qps = P.ps("qps", [128, 2, 512], F32)
                    rps_ = P.ps("qrps", [128, 2, 512], F32)
                    ui = 0
                    for hh in range(cfg.NQH + cfg.NKV):
                        isq = hh < cfg.NQH
                        row0 = (4 * 128 + hh * 128) if isq else (KVO + (hh - cfg.NQH) * 128)
                        dstT = qT if isq else kT
                        hidx = hh if isq else hh - cfg.NQH
                        gcol = 0 if isq else 1
                        for (t0, n) in all_tiles:
                            u = ui % 2
                            ui += 1
                            P.dma("sync", raw[u][:, :n], pT_d[row0:row0 + 128, t0:t0 + n], (), [f"qraw{u}"])
                            P.act(sq[u][:, :n], raw[u][:, :n], AF.Square, [f"qraw{u}"], [f"qsq{u}"])
                            P.mm(qps[:, u, :n], ones_f[:], sq[u][:, :n], True, True, ["ones_f", f"qsq{u}"], [f"qps{u}"])
                            P.ts("dve", rb[u][:, :n], qps[:, u, :n], 1.0 / cfg.HD, cfg.EPS, ALU.mult, ALU.add, [f"qps{u}"], [f"qrb{u}"])
                            P.act(rb[u][:, :n], rb[u][:, :n], AF.Sqrt, [f"qrb{u}"], [f"qrb{u}"])
                            P.emit("dve", lambda e, a=rb[u][:, :n]: e.reciprocal(out=a, in_=a), [f"qrb{u}"], [f"qrb{u}"])
                            P.stt(qn[u][:, :n], raw[u][:, :n], qg_all[:, gcol, i:i + 1], rb[u][:, :n], ALU.mult, ALU.mult,
                                  [f"qraw{u}", "qg", f"qrb{u}"], [f"qn{u}"])
                            if t0 >= L:
                                P.dma("act", rC[u][:, :n], I["ropeC"][:, t0 - L:t0 - L + n], (), [f"rC{u}"])
                                P.dma("act", rS[u][:, :n], I["ropeS"][:, t0 - L:t0 - L + n], (), [f"rS{u}"])
                                P.mm(rps_[:, u, :n], rR[:], qn[u][:, :n], True, True, ["rR", f"qn{u}"], [f"qrps{u}"])
                                P.tt("dve", t1[u][:, :n], rps_[:, u, :n], rS[u][:, :n], ALU.mult,
                                     [f"qrps{u}", f"rS{u}"], [f"qt1{u}"])
                                P.tt("pool", t2[u][:, :n], qn[u][:, :n], rC[u][:, :n], ALU.mult,
                                     [f"qn{u}", f"rC{u}"], [f"qt2{u}"])
                                P.tt("dve", dstT[:, hidx, t0:t0 + n], t1[u][:, :n], t2[u][:, :n], ALU.add,
                                     [f"qt1{u}", f"qt2{u}"], ["qT" if isq else "kT"])
                            else:
                                P.copy("pool", dstT[:, hidx, t0:t0 + n], qn[u][:, :n], [f"qn{u}"], ["qT" if isq else "kT"])
                with P.phase():
                    NKMAX = T
                    att_s = P.ps("att_s", [128, 5, 512], F32)
                    att_t = P.ps("att_t", [128, 2, 8, 128], BF16)
                    att_o = P.ps("att_o", [128, 128], F32)
                    pbuf = [P.sb(f"pbuf{j}", [128, NKMAX], BF16) for j in range(2)]
                    pTb = [P.sb(f"pTb{j}", [128, TCH, 128], BF16) for j in range(2)]
                    mx = [P.sb(f"amx{j}", [128, 4], F32) for j in range(2)]
                    s_flat = att_s[:, :, :].rearrange("p a b -> p (a b)")
                    qtiles = [(t0, 0, T) for t0 in range(L, T, 128)]
                    if not last:
                        qtiles += [(t0, 0, L) for t0 in range(0, L, 128)]
                    ui = 0
                    gcount = 0
                    for (q0, klo, khi) in qtiles:
                        NK = khi - klo
                        nch = NK // 128
                        for h in range(cfg.NQH):
                            g = h // (cfg.NQH // cfg.NKV)
                            u = ui % 2
                            ui += 1
                            for kt, k0 in enumerate(range(klo, khi, 512)):
                                n = min(512, khi - k0)
                                P.mm(att_s[:, kt, :n], qT[:, h, q0:q0 + 128], kT[:, g, k0:k0 + n], True, True,
                                     ["qT", "kT"], ["att_s"])
                            P.emit("dve", lambda e, o=mx[u][:, 0:1], a=s_flat[:, :NK]: e.reduce_max(out=o, in_=a, axis=AX.X),
                                   ["att_s"], [f"amx{u}"])
                            P.ts("dve", mx[u][:, 1:2], mx[u][:, 0:1], -scale, None, ALU.mult, None, [f"amx{u}"], [f"amx{u}"])
                            P.act(pbuf[u][:, :NK], s_flat[:, :NK], AF.Exp, ["att_s", f"amx{u}"], [f"pbuf{u}", f"amx{u}"],
                                  bias=mx[u][:, 1:2], scale=scale, accum_out=mx[u][:, 2:3])
                            P.emit("dve", lambda e, o=mx[u][:, 3:4], a=mx[u][:, 2:3]: e.reciprocal(out=o, in_=a),
                                   [f"amx{u}"], [f"amx{u}"])
                            P.ts("dve", pbuf[u][:, :NK], pbuf[u][:, :NK], mx[u][:, 3:4], None, ALU.mult, None,
                                 [f"pbuf{u}", f"amx{u}"], [f"pbuf{u}"])
                            for c0 in range(0, nch, 8):
                                cn = min(8, nch - c0)
                                gb = gcount % 2
                                gcount += 1
                                for c in range(cn):
                                    P.tr(att_t[:, gb, c, :], pbuf[u][:, (c0 + c) * 128:(c0 + c + 1) * 128], ident_b[:],
                                         [f"pbuf{u}", "ident_b"], [f"att_t{gb}"])
                                P.copy("act", pTb[u][:, c0:c0 + cn, :], att_t[:, gb, 0:cn, :], [f"att_t{gb}"], [f"pTb{u}"])
                            for c in range(nch):
                                P.mm(att_o[:, :], vtok[:, klo // 128 + c, g * 128:(g + 1) * 128], pTb[u][:, c, :],
                                     c == 0, c == nch - 1, ["vtok", f"pTb{u}"], ["att_o"])
                            P.copy("act", catT[:, 4 + h, q0:q0 + 128], att_o[:, :], ["att_o"], [f"cat{4 + h}"])
            with P.phase():
                ev = residual_evac(l, 2, "oo")
                linear("owout", catT, [f"cat{m}" for m in range(KC)], KC, I["od_w_out"][i], 0, D, out_tiles, ev)

    def moe(l):
        last = (l == DEPTH - 1)
        lo = L if last else 0
        sts = []
        t = lo
        while t < T:
            hi = min(T, t + 768)
            sts.append((t, hi))
            t = hi
        for (s0, s1) in sts:
            NT = s1 - s0
            ntiles = tok_tiles(s0, s1, L, 384 if (s0 >= L or s1 <= L) else 512)
            with P.phase():
                hT = P.sb("mhT", [128, KC, NT], BF16)
                GT = P.sb("mGT", [E, NT], F32)
                acc = P.sb("macc", [128, KC, NT], F32)
                with P.phase():
                    lg = [P.sb(f"lg{j}", [128, E], F32) for j in range(2)]
                    mk = [P.sb(f"mk{j}", [128, E], F32) for j in range(2)]
                    ex = [P.sb(f"ex{j}", [128, E], F32) for j in range(2)]
                    m8 = [P.sb(f"m8{j}", [128, 8], F32) for j in range(2)]
                    sm = [P.sb(f"sm{j}", [128, 2], F32) for j in range(2)]
                    gtp = P.ps("gtp", [E, 2, 512], F32)
                    cnt = {"i": 0}

                    def rtile(ti, t0, n, h32, rw, rbias, rps):
                        for j in range(n // 128):
                            q = cnt["i"] % 2
                            cnt["i"] += 1
                            slot = j % 2
                            for k in range(KC):
                                P.mm(rps[:, slot, :E], h32[:, k, j * 128:(j + 1) * 128], rw[:, k, :], k == 0, False,
                                     ["h32", "rw"], [f"rps{slot}"])
                            P.mm(rps[:, slot, :E], ones_f[0:1, :], rbias[0:1, :], False, True, ["ones_f", "rbias"], [f"rps{slot}"])
                            P.copy("dve", lg[q][:], rps[:, slot, :E], [f"rps{slot}"], [f"lg{q}"])
                            P.emit("dve", lambda e, o=m8[q][:], a=lg[q][:]: e.max(out=o, in_=a), [f"lg{q}"], [f"m8{q}"])
                            P.ts("dve", mk[q][:], lg[q][:], m8[q][:, cfg.TOPK - 1:cfg.TOPK], None, ALU.is_ge, None,
                                 [f"lg{q}", f"m8{q}"], [f"mk{q}"])
                            P.ts("dve", sm[q][:, 0:1], m8[q][:, 0:1], -1.0, None, ALU.mult, None, [f"m8{q}"], [f"sm{q}"])
                            P.act(ex[q][:], lg[q][:], AF.Exp, [f"lg{q}", f"sm{q}"], [f"ex{q}"], bias=sm[q][:, 0:1])
                            P.tt("dve", ex[q][:], ex[q][:], mk[q][:], ALU.mult, [f"ex{q}", f"mk{q}"], [f"ex{q}"])
                            P.emit("dve", lambda e, o=sm[q][:, 1:2], a=ex[q][:]: e.reduce_sum(out=o, in_=a, axis=AX.X),
                                   [f"ex{q}", f"sm{q}"], [f"sm{q}"])
                            P.emit("dve", lambda e, a=sm[q][:, 1:2]: e.reciprocal(out=a, in_=a), [f"sm{q}"], [f"sm{q}"])
                            P.ts("dve", ex[q][:], ex[q][:], sm[q][:, 1:2], None, ALU.mult, None, [f"ex{q}", f"sm{q}"], [f"ex{q}"])
                            P.tr(gtp[:, q, :128], ex[q][:], ident_f[:], [f"ex{q}", "ident_f"], [f"gtp{q}"])
                            c0 = t0 - s0 + j * 128
                            P.copy("act", GT[:, c0:c0 + 128], gtp[:, q, :128], [f"gtp{q}"], ["mGT"])
                    norm_phase(l, 1, hT_view(hT, s0), ntiles, gffn, router={"tile": rtile})
                    P.dma("sync", gT_d[:, s0:s1], GT[:, :], ["mGT"], ["gT_d"])
                hreads = [f"hT{ti}" for ti in range(len(ntiles))]
                with P.phase():
                    stg = [P.sb(f"xst{j}", [128, 2048], F32) for j in range(3)]
                    wgb = [P.sb(f"wgb{j}", [128, KC, 128], BF16) for j in range(2)]
                    wub = [P.sb(f"wub{j}", [128, KC, 128], BF16) for j in range(2)]
                    wdb = [P.sb(f"wdb{j}", [128, FC, 512], BF16) for j in range(3)]
                    aT = [P.sb(f"aT{j}", [128, FC, NT], BF16) for j in range(2)]
                    gbc = [P.sb(f"gbc{j}", [128, NT], F32) for j in range(2)]
                    bg = P.sb("bg", [128, E * FC], F32)
                    bu = P.sb("bu", [128, E * FC], F32)
                    bd = P.sb("bd", [E, D], F32)
                    P.dma("sync", bg[:], I["b_gateT"][:, l * E * FC:(l + 1) * E * FC], (), ["bg"])
                    P.dma("sync", bu[:], I["b_upT"][:, l * E * FC:(l + 1) * E * FC], (), ["bu"])
                    P.dma("sync", bd[:], I["b_down"][l * E:(l + 1) * E, :], (), ["bd"])
                    gps = P.ps("gps", [128, 2, 512], F32)
                    ups = P.ps("ups", [128, 2, 512], F32)
                    dps = P.ps("dps", [128, 3, 512], F32)
                    tg = [P.sb(f"tg{j}", [128, 512], F32) for j in range(2)]
                    tu = [P.sb(f"tu{j}", [128, 512], F32) for j in range(2)]
                    tsg = [P.sb(f"tsg{j}", [128, 512], F32) for j in range(2)]
                    cnt_ = {"si": 0, "ei": 0, "di": 0}
                    items = []
                    for e_ in range(E):
                        for fm in range(FC):
                            items.append(("gu", e_, fm))
                        for dq in range(D // 512):
                            items.append(("d", e_, dq))

                    def wload(it):
                        kind, e_, j = it
                        ge = l * E + e_
                        if kind == "gu":
                            wb = (e_ * FC + j) % 2
                            for (wsrc, wdst, nm) in ((I["w_gate"], wgb, "wgb"), (I["w_up"], wub, "wub")):
                                sb_ = cnt_["si"] % 3
                                cnt_["si"] += 1
                                sv = stg[sb_][:, :].rearrange("p (k n) -> p k n", k=KC)
                                P.dma("sync", sv, wsrc[ge, :, j * 128:(j + 1) * 128].rearrange("(k p) n -> p k n", p=128),
                                      (), [f"xst{sb_}"])
                                P.copy("act", wdst[wb][:], sv, [f"xst{sb_}"], [f"{nm}{wb}"])
                        else:
                            wq = (e_ * (D // 512) + j) % 3
                            sb_ = cnt_["si"] % 3
                            cnt_["si"] += 1
                            sv = stg[sb_][:, :].rearrange("p (k n) -> p k n", k=FC)
                            P.dma("sync", sv, I["w_down"][ge, :, j * 512:(j + 1) * 512].rearrange("(k p) n -> p k n", p=128),
                                  (), [f"xst{sb_}"])
                            P.copy("act", wdb[wq][:], sv, [f"xst{sb_}"], [f"wdb{wq}"])

                    def wcompute(it):
                        kind, e_, j = it
                        ab = e_ % 2
                        if kind == "gu":
                            fm = j
                            wb = (e_ * FC + fm) % 2
                            if fm == 0:
                                P.dma("pool", gbc[ab][:], gT_d[e_:e_ + 1, s0:s1].partition_broadcast(128), ["gT_d"], [f"gbc{ab}"])
                            for (t0, n) in ntiles:
                                q = cnt_["ei"] % 2
                                cnt_["ei"] += 1
                                c0 = t0 - s0
                                for k in range(KC):
                                    P.mm(gps[:, q, :n], wgb[wb][:, k, :], hT[:, k, c0:c0 + n], k == 0, k == KC - 1,
                                         [f"wgb{wb}"] + hreads, [f"gps{q}"], lazy=True)
                                for k in range(KC):
                                    P.mm(ups[:, q, :n], wub[wb][:, k, :], hT[:, k, c0:c0 + n], k == 0, k == KC - 1,
                                         [f"wub{wb}"] + hreads, [f"ups{q}"], lazy=True)
                                bi = e_ * FC + fm
                                P.ts("dve", tg[q][:, :n], gps[:, q, :n], bg[:, bi:bi + 1], cfg.LIMIT, ALU.add, ALU.min,
                                     [f"gps{q}", "bg"], [f"tg{q}"])
                                P.ts("dve", tu[q][:, :n], ups[:, q, :n], bu[:, bi:bi + 1], cfg.LIMIT, ALU.add, ALU.min,
                                     [f"ups{q}", "bu"], [f"tu{q}"])
                                P.ts("dve", tu[q][:, :n], tu[q][:, :n], -cfg.LIMIT, 1.0, ALU.max, ALU.add, [f"tu{q}"], [f"tu{q}"])
                                P.act(tsg[q][:, :n], tg[q][:, :n], AF.Sigmoid, [f"tg{q}"], [f"tsg{q}"], scale=cfg.ALPHA)
                                P.tt("pool", tu[q][:, :n], tu[q][:, :n], gbc[ab][:, c0:c0 + n], ALU.mult,
                                     [f"tu{q}", f"gbc{ab}"], [f"tu{q}"])
                                P.tt("dve", tg[q][:, :n], tg[q][:, :n], tsg[q][:, :n], ALU.mult, [f"tg{q}", f"tsg{q}"], [f"tg{q}"])
                                P.tt("dve", aT[ab][:, fm, c0:c0 + n], tg[q][:, :n], tu[q][:, :n], ALU.mult,
                                     [f"tg{q}", f"tu{q}"], [f"aT{ab}"])
                        else:
                            dq = j
                            wq = (e_ * (D // 512) + dq) % 3
                            for mi in range(4):
                                m = dq * 4 + mi
                                for (t0, n) in ntiles:
                                    q = cnt_["di"] % 3
                                    cnt_["di"] += 1
                                    c0 = t0 - s0
                                    for kf in range(FC):
                                        P.mm(dps[:, q, :n], wdb[wq][:, kf, mi * 128:(mi + 1) * 128], aT[ab][:, kf, c0:c0 + n],
                                             kf == 0, kf == FC - 1 and e_ != 0, [f"wdb{wq}", f"aT{ab}"], [f"dps{q}"], lazy=True)
                                    if e_ == 0:
                                        P.mm(dps[:, q, :n], bd[:, m * 128:(m + 1) * 128], GT[:, c0:c0 + n], False, True,
                                             ["bd", "mGT"], [f"dps{q}"])
                                        P.copy("dve", acc[:, m, c0:c0 + n], dps[:, q, :n], [f"dps{q}"], [f"acc{m}"])
                                    else:
                                        P.tt("dve", acc[:, m, c0:c0 + n], acc[:, m, c0:c0 + n], dps[:, q, :n], ALU.add,
                                             [f"acc{m}", f"dps{q}"], [f"acc{m}"])
                    wload(items[0])
                    for ii, it in enumerate(items):
                        if ii + 1 < len(items):
                            wload(items[ii + 1])
                        wcompute(it)
                with P.phase():
                    xo = [P.sb(f"mxo{j}", [128, 512], F32) for j in range(3)]
                    ri = 0
                    for m in range(KC):
                        for (t0, n) in ntiles:
                            j = ri % 3
                            ri += 1
                            c = 0 if t0 >= L else 1
                            c0 = t0 - s0
                            P.dma("sync", xo[j][:, :n], xs[m * 128:(m + 1) * 128, t0:t0 + n], (), [f"mxo{j}"])
                            P.stt(xo[j][:, :n], acc[:, m, c0:c0 + n], mod(l, 5, m, c), xo[j][:, :n], ALU.mult, ALU.add,
                                  [f"acc{m}", "modT", f"mxo{j}"], [f"mxo{j}"])
                            P.dma("pool", xs[m * 128:(m + 1) * 128, t0:t0 + n], xo[j][:, :n], [f"mxo{j}"], [f"xsw{m}"])

    class hT_view:
        def __init__(self, t, off):
            self.t, self.off = t, off

        def __getitem__(self, key):
            a, k, sl = key
            return self.t[a, k, slice(sl.start - self.off, sl.stop - self.off)]

    stop_after = getattr(cfg, "STOP", None)
    try:
        for l in range(DEPTH):
            if stop_after is not None and l >= stop_after:
                break
            if l % 2 == 0:
                even_mixer(l)
            else:
                odd_mixer(l)
            if STAGE == "MIX":
                raise StopBuild()
            moe(l)
    except StopBuild:
        pass

    with P.phase():
        tiles = tok_tiles(L, T, L)
        xt = [P.sb(f"fx{i}", [128, KC, 512], F32) for i in range(2)]
        sq = [P.sb(f"fsq{i}", [128, 512], F32) for i in range(2)]
        rb = [P.sb(f"frb{i}", [128, 512], F32) for i in range(2)]
        ot = [P.sb(f"fo{i}", [128, 512], F32) for i in range(3)]
        nps = P.ps("fps_", [128, 2, 512], F32)
        oi = 0
        for ti, (t0, n) in enumerate(tiles):
            b = ti % 2
            P.dma("sync", xt[b][:, :, :n], xs[:, t0:t0 + n].rearrange("(k p) t -> p k t", p=128), XS_ALL, [f"fx{b}"])
            for k in range(KC):
                s = sq[k % 2]
                P.act(s[:, :n], xt[b][:, k, :n], AF.Square, [f"fx{b}"], [f"fsq{k % 2}"])
                P.mm(nps[:, b, :n], ones_f[:], s[:, :n], k == 0, k == KC - 1, ["ones_f", f"fsq{k % 2}"], [f"fnps{b}"])
            P.ts("dve", rb[b][:, :n], nps[:, b, :n], 1.0 / D, cfg.EPS, ALU.mult, ALU.add, [f"fnps{b}"], [f"frb{b}"])
            P.act(rb[b][:, :n], rb[b][:, :n], AF.Sqrt, [f"frb{b}"], [f"frb{b}"])
            P.emit("dve", lambda e, a=rb[b][:, :n]: e.reciprocal(out=a, in_=a), [f"frb{b}"], [f"frb{b}"])
            for k in range(KC):
                o = oi % 3
                oi += 1
                P.stt(ot[o][:, :n], xt[b][:, k, :n], gfin[:, k:k + 1], rb[b][:, :n], ALU.mult, ALU.mult,
                      [f"fx{b}", "gfin", f"frb{b}"], [f"fo{o}"])
                P.dma("pool", outT[k * 128:(k + 1) * 128, t0 - L:t0 - L + n], ot[o][:, :n], [f"fo{o}"], ["outT"])
    return P


def _pT(v, nchunk):
    v = np.asarray(v, np.float32)
    lead = v.shape[:-1]
    r = v.reshape(*lead, nchunk, 128)
    r = np.moveaxis(r, -1, 0)
    return np.ascontiguousarray(r.reshape(128, -1))


def const_tables(cfg):
    S, L = cfg.S, cfg.L
    out = {}

    def dft(n, scale):
        k = np.arange(n, dtype=np.float64)
        ang = 2.0 * np.pi * np.outer(k, k) / n
        return (np.cos(ang) * scale).astype(np.float32), (-np.sin(ang) * scale).astype(np.float32)
    c, s = dft(128, 128 ** -0.5)
    out["dft_d"] = np.concatenate([c, -s], axis=1)
    out["dftc_lat"], out["dfts_lat"] = dft(S, S ** -0.5)
    out["dftc_ctx"], out["dfts_ctx"] = dft(L, L ** -0.5)
    t = np.arange(S)
    pos = np.stack([t // cfg.GRID_W, t % cfg.GRID_W], axis=0).astype(np.float32)
    inv = np.power(np.float32(cfg.ROPE_THETA), -np.arange(32, dtype=np.float32) / np.float32(32)).astype(np.float32)
    ang = (pos[:, None, :] * inv[None, :, None]).astype(np.float32)
    C = np.zeros((128, S), np.float32)
    Sg = np.zeros((128, S), np.float32)
    for ax in range(2):
        for half in range(2):
            C[ax * 64 + half * 32:ax * 64 + half * 32 + 32] = np.cos(ang[ax])
            Sg[ax * 64 + half * 32:ax * 64 + half * 32 + 32] = np.sin(ang[ax])
    out["ropeC"], out["ropeS"] = C, Sg
    R = np.zeros((128, 128), np.float32)
    for m in range(128):
        if (m % 64) < 32:
            R[m, m + 32] = -1.0
        else:
            R[m, m - 32] = 1.0
    out["ropeR"] = np.ascontiguousarray(R.T)
    rc = np.zeros((8, max(S, L)), np.float32)
    for gi, w in enumerate(cfg.POOL_WINDOWS):
        for r, n in enumerate((L, S)):
            tt_ = np.arange(n)
            lo = np.clip(tt_ - w // 2, 0, n)
            hi = np.clip(tt_ + w - w // 2, 0, n)
            rc[gi * 2 + r, :n] = (1.0 / (hi - lo).astype(np.float32)).astype(np.float32)
    out["pool_rc"] = rc
    return out


def prepare_inputs(cfg, inp, b):
    D, KC, DEPTH, NE, NO, E = cfg.D, cfg.KC, cfg.DEPTH, cfg.NE, cfg.NO, cfg.E
    NPAIR = cfg.NG // 2
    f = lambda a: np.ascontiguousarray(np.asarray(a, np.float32))
    m = {}
    m["xT"] = f(np.concatenate([inp["ctx"][b], inp["x"][b]], axis=0).T)
    cond = np.stack([inp["c"][b], inp["c_ctx"]], axis=0)
    m["condT"] = f(cond.reshape(2, KC, 128).transpose(2, 1, 0).reshape(128, KC * 2))
    m["ada_w"] = f(inp["ada_w"])
    m["ada_bT"] = f(inp["ada_b"].reshape(DEPTH, 96, 128).transpose(2, 0, 1).reshape(128, DEPTH * 96))
    m["gmixT"] = f(inp["norm_mix_g"].reshape(DEPTH, KC, 128).transpose(2, 0, 1).reshape(128, DEPTH * KC))
    m["gffnT"] = f(inp["norm_ffn_g"].reshape(DEPTH, KC, 128).transpose(2, 0, 1).reshape(128, DEPTH * KC))
    m["finalgT"] = f(inp["final_g"].reshape(KC, 128).T)
    if NE:
        m["ev_w_in"] = f(inp["ev_w_in"])
        m["ev_w_out"] = f(inp["ev_w_out"])

        def pairT(a):
            a = np.asarray(a, np.float32).reshape(NE, 2, NPAIR, 2, cfg.NP)
            return f(a.transpose(3, 4, 0, 1, 2).reshape(128, NE * 2 * NPAIR))
        m["lamreT"] = pairT(inp["s5_lam_re"])
        m["lamimT"] = pairT(inp["s5_lam_im"])
        m["logdtT"] = pairT(np.broadcast_to(np.asarray(inp["s5_log_dt"])[..., None], (NE, 2, cfg.NG, cfg.NP)))
        for nm, src in (("bblk_re", inp["s5_b_re"]), ("bblk_im", inp["s5_b_im"])):
            a = np.asarray(src, np.float32).reshape(NE, 2, NPAIR, 2, cfg.NP, 16)
            blk = np.zeros((NE, 2, NPAIR, 128, 128), np.float32)
            for gp in range(NPAIR):
                for j in range(2):
                    r0 = ((2 * gp + j) % 8) * 16
                    blk[:, :, gp, r0:r0 + 16, j * 64:(j + 1) * 64] = a[:, :, gp, j].transpose(0, 1, 3, 2)
            m[nm] = f(blk.reshape(NE * 2 * NPAIR, 128, 128))
        for nm, src in (("cblk_re", inp["s5_c_re"]), ("cblk_im", inp["s5_c_im"])):
            a = np.asarray(src, np.float32).reshape(NE, 2, NPAIR, 2, 16, cfg.NP)
            blk = np.zeros((NE, 2, NPAIR, 128, 32), np.float32)
            for j in range(2):
                blk[:, :, :, j * 64:(j + 1) * 64, j * 16:(j + 1) * 16] = a[:, :, :, j].transpose(0, 1, 2, 4, 3)
            m[nm] = f(blk.reshape(NE * 2 * NPAIR, 128, 32))
        SC = cfg.S5W // 128
        m["s5dT"] = f(inp["s5_d"].reshape(NE, SC, 128).transpose(2, 0, 1).reshape(128, NE * SC))
        m["glu_w"] = f(inp["s5_glu_w"])
        m["glu_bT"] = f(inp["s5_glu_b"].reshape(NE, SC, 128).transpose(2, 0, 1).reshape(128, NE * SC))
        m["fnet_w"] = f(inp["fnet_w"].reshape(NE * 4, 128, 128))
    if NO:
        m["od_w_in"] = f(inp["od_w_in"])
        m["od_w_out"] = f(inp["od_w_out"])
        m["pool_w"] = f(inp["pool_w"].reshape(NO * 4, 128, 128))
        m["pool_scaleT"] = f(inp["pool_scale"].reshape(NO, 4, 128).transpose(2, 0, 1).reshape(128, NO * 4))
        m["qgainT"] = f(np.asarray(inp["q_gain"]).T)
        m["kgainT"] = f(np.asarray(inp["k_gain"]).T)
    m["router_w"] = f(inp["router_w"])
    m["router_b"] = f(inp["router_b"].reshape(1, DEPTH * E))
    m["w_gate"] = f(inp["exp_w_gate"].reshape(DEPTH * E, D, cfg.FF))
    m["w_up"] = f(inp["exp_w_up"].reshape(DEPTH * E, D, cfg.FF))
    m["w_down"] = f(inp["exp_w_down"].reshape(DEPTH * E, cfg.FF, D))
    FC = cfg.FF // 128
    m["b_gateT"] = f(inp["exp_b_gate"].reshape(DEPTH * E, FC, 128).transpose(2, 0, 1).reshape(128, DEPTH * E * FC))
    m["b_upT"] = f(inp["exp_b_up"].reshape(DEPTH * E, FC, 128).transpose(2, 0, 1).reshape(128, DEPTH * E * FC))
    m["b_down"] = f(inp["exp_b_down"].reshape(DEPTH * E, D))
    return m


def run(cfg, inp):
    P = build(cfg)
    consts = const_tables(cfg)
    declared = set()
    for alloc in P.nc.allocations:
        try:
            if alloc.kind == "ExternalInput":
                declared.add(alloc.memorylocations[0].name)
        except Exception:
            pass
    in_maps = []
    for b in range(cfg.B):
        m = prepare_inputs(cfg, inp, b)
        m.update(consts)
        in_maps.append({k: v for k, v in m.items() if k in declared})
    res = run_bass_kernel_spmd(P.nc, in_maps, core_ids=list(range(cfg.B)))
    out = np.stack([np.ascontiguousarray(res.results[b]["outT"].T) for b in range(cfg.B)], axis=0)
    return out.astype(np.float32)


def kernel(**inputs):
    return run(Cfg(), inputs)
```
